# Optimizing a Trainium2 kernel written in Bass

```python
import jax, jax.numpy as jnp
from jax import lax
import numpy as np

D_MODEL = 1024
BATCH = 8
SEQ = 4096
DEPTH = 2

RET_HEADS = 4
RET_DIM = 128
D_RET = RET_HEADS * RET_DIM
MLSTM_HEADS = 4
MLSTM_DIM = 128
D_MLSTM = MLSTM_HEADS * MLSTM_DIM
D_MIX = D_RET + D_MLSTM
D_FF = 2816
CHUNK = 128
CONV_WIDTH = 4
ROPE_BASE = 10000.0
EPS = 1e-6
N_SUBLAYERS = 3
D_IN = 4 * D_RET + 4 * D_MLSTM + 2 * MLSTM_HEADS

kernel_name = "hybrid_retention_mlstm_macaron_adaln"


def rmsnorm(x, g):
    xf = x.astype(jnp.float32)
    xf = xf * lax.rsqrt(jnp.mean(xf * xf, axis=-1, keepdims=True) + EPS)
    return xf.astype(x.dtype) * g


def ada_mod(c, w, b):
    mod = jax.nn.silu(c) @ w + b
    shift, scale, gate = jnp.split(mod, 3, axis=-1)
    return shift[:, None, :], scale[:, None, :], gate[:, None, :]


def swiglu(h, w1, w3, w2):
    return (jax.nn.silu(h @ w1) * (h @ w3)) @ w2


def to_heads(t, n_heads):
    b, s, _ = t.shape
    return t.reshape(b, s, n_heads, -1).transpose(0, 2, 1, 3).astype(jnp.float32)


def head_norm(h, g):
    mu = jnp.mean(h, axis=-1, keepdims=True)
    var = jnp.mean(jnp.square(h - mu), axis=-1, keepdims=True)
    hn = (h - mu) * lax.rsqrt(var + EPS)
    b, nh, s, dh = hn.shape
    return hn.transpose(0, 2, 1, 3).reshape(b, s, nh * dh) * g.astype(jnp.float32)


def rotary(t, positions):
    dh = t.shape[-1]
    inv_freq = ROPE_BASE ** (-jnp.arange(0, dh, 2, dtype=jnp.float32) / dh)
    ang = positions.astype(jnp.float32)[:, None, :, None] * inv_freq
    cos, sin = jnp.cos(ang), jnp.sin(ang)
    t1, t2 = jnp.split(t, 2, axis=-1)
    return jnp.concatenate([t1 * cos - t2 * sin, t1 * sin + t2 * cos], axis=-1)


def causal_depthwise_conv(u, w, b):
    s = u.shape[1]
    up = jnp.pad(u, ((0, 0), (CONV_WIDTH - 1, 0), (0, 0)))
    out = b
    for j in range(CONV_WIDTH):
        out = out + w[j] * up[:, j:j + s, :]
    return out


def retention_chunkwise(q, k, v):
    b, nh, s, dh = q.shape
    nc = s // CHUNK
    k = k * (dh ** -0.5)
    log_gamma = jnp.log(1.0 - 2.0 ** (-5.0 - jnp.arange(nh, dtype=jnp.float32)))
    idx = jnp.arange(CHUNK)
    diff = (idx[:, None] - idx[None, :]).astype(jnp.float32)
    decay = jnp.where(diff >= 0, jnp.exp(log_gamma[:, None, None] * jnp.maximum(diff, 0.0)), 0.0)
    qc = q.reshape(b, nh, nc, CHUNK, dh)
    kc = k.reshape(b, nh, nc, CHUNK, dh)
    vc = v.reshape(b, nh, nc, CHUNK, dh)
    scores = jnp.einsum('bhcld,bhcmd->bhclm', qc, kc) * decay[None, :, None]
    o_intra = jnp.einsum('bhclm,bhcmd->bhcld', scores, vc)
    w_k = jnp.exp(log_gamma[:, None] * (CHUNK - 1 - idx).astype(jnp.float32))
    kv = jnp.einsum('bhcld,bhcle->bhcde', kc * w_k[None, :, None, :, None], vc)
    chunk_decay = jnp.exp(log_gamma * CHUNK)[:, None, None]

    def step(state, kv_c):
        return state * chunk_decay + kv_c, state

    _, prev = lax.scan(step, jnp.zeros((b, nh, dh, dh), jnp.float32), jnp.moveaxis(kv, 2, 0))
    prev = jnp.moveaxis(prev, 0, 2)
    w_q = jnp.exp(log_gamma[:, None] * (idx + 1).astype(jnp.float32))
    o_inter = jnp.einsum('bhcld,bhcde->bhcle', qc * w_q[None, :, None, :, None], prev)
    return (o_intra + o_inter).reshape(b, nh, s, dh)


def mlstm_chunkwise(q, k, v, i_pre, f_pre):
    b, nh, s, dh = q.shape
    nc = s // CHUNK
    q = q * (dh ** -0.5)
    log_f = jax.nn.log_sigmoid(f_pre)
    chunks = lambda t: jnp.moveaxis(t.reshape(b, nh, nc, CHUNK, *t.shape[3:]), 2, 0)
    causal = jnp.tril(jnp.ones((CHUNK, CHUNK), dtype=bool))

    def step(carry, inp):
        C, n, m = carry
        qc, kc, vc, ic, lfc = inp
        bcum = jnp.cumsum(lfc, axis=-1)
        log_d = jnp.where(causal, bcum[..., :, None] - bcum[..., None, :] + ic[..., None, :], -jnp.inf)
        inter_log = bcum + m[..., None]
        m_t = jnp.maximum(jnp.max(log_d, axis=-1), inter_log)
        w_d = jnp.exp(log_d - m_t[..., None])
        w_inter = jnp.exp(inter_log - m_t)
        sc = jnp.einsum('bhld,bhmd->bhlm', qc, kc) * w_d
        num = jnp.einsum('bhlm,bhmd->bhld', sc, vc) + w_inter[..., None] * jnp.einsum('bhld,bhde->bhle', qc, C)
        den = jnp.sum(sc, axis=-1) + w_inter * jnp.einsum('bhld,bhd->bhl', qc, n)
        h = num / jnp.maximum(jnp.abs(den), jnp.exp(-m_t))[..., None]
        b_last = bcum[..., -1]
        g = b_last[..., None] - bcum + ic
        m_new = jnp.maximum(b_last + m, jnp.max(g, axis=-1))
        w_c = jnp.exp(b_last + m - m_new)
        w_g = jnp.exp(g - m_new[..., None])
        C_new = w_c[..., None, None] * C + jnp.einsum('bhld,bhle->bhde', kc * w_g[..., None], vc)
        n_new = w_c[..., None] * n + jnp.einsum('bhl,bhld->bhd', w_g, kc)
        return (C_new, n_new, m_new), h

    init = (jnp.zeros((b, nh, dh, dh), jnp.float32), jnp.zeros((b, nh, dh), jnp.float32),
            jnp.zeros((b, nh), jnp.float32))
    _, hs = lax.scan(step, init, (chunks(q), chunks(k), chunks(v), chunks(i_pre), chunks(log_f)))
    return jnp.moveaxis(hs, 0, 2).reshape(b, nh, s, dh)


def setup_inputs(seed: int = 0) -> dict:
    key = jax.random.key(seed)
    ks = jax.random.split(key, 20)
    f32 = jnp.float32
    nrm = lambda k, shape, fan_in: jax.random.normal(k, shape, f32) * (fan_in ** -0.5)
    x = jax.random.normal(ks[0], (BATCH, SEQ, D_MODEL), f32)
    c = jax.random.normal(ks[1], (BATCH, D_MODEL), f32)
    offsets = jax.random.randint(ks[2], (BATCH, 1), 0, SEQ, dtype=jnp.int32)
    positions = offsets + jnp.arange(SEQ, dtype=jnp.int32)[None, :]
    norm_g = 1.0 + 0.02 * jax.random.normal(ks[3], (DEPTH, N_SUBLAYERS, D_MODEL), f32)
    w_ada = nrm(ks[4], (DEPTH, N_SUBLAYERS, D_MODEL, 3 * D_MODEL), D_MODEL)
    b_ada = 0.02 * jax.random.normal(ks[5], (DEPTH, N_SUBLAYERS, 3 * D_MODEL), f32)
    w_ff1 = nrm(ks[6], (DEPTH, 2, D_MODEL, D_FF), D_MODEL)
    w_ff3 = nrm(ks[7], (DEPTH, 2, D_MODEL, D_FF), D_MODEL)
    w_ff2 = nrm(ks[8], (DEPTH, 2, D_FF, D_MODEL), D_FF)
    w_in = nrm(ks[9], (DEPTH, D_MODEL, D_IN), D_MODEL)
    conv_w = nrm(ks[10], (DEPTH, CONV_WIDTH, 2 * D_MLSTM), CONV_WIDTH)
    conv_b = 0.02 * jax.random.normal(ks[11], (DEPTH, 2 * D_MLSTM), f32)
    b_igate = 0.1 * jax.random.normal(ks[12], (DEPTH, MLSTM_HEADS), f32)
    b_fgate = (jnp.linspace(3.0, 6.0, MLSTM_HEADS, dtype=f32)[None, :]
               + 0.1 * jax.random.normal(ks[13], (DEPTH, MLSTM_HEADS), f32))
    g_ret_norm = 1.0 + 0.02 * jax.random.normal(ks[14], (DEPTH, D_RET), f32)
    g_mlstm_norm = 1.0 + 0.02 * jax.random.normal(ks[15], (DEPTH, D_MLSTM), f32)
    w_out = nrm(ks[16], (DEPTH, D_MIX, D_MODEL), D_MIX)
    g_final = 1.0 + 0.02 * jax.random.normal(ks[17], (D_MODEL,), f32)
    return {"x": x, "c": c, "positions": positions, "norm_g": norm_g, "w_ada": w_ada,
            "b_ada": b_ada, "w_ff1": w_ff1, "w_ff3": w_ff3, "w_ff2": w_ff2, "w_in": w_in,
            "conv_w": conv_w, "conv_b": conv_b, "b_igate": b_igate, "b_fgate": b_fgate,
            "g_ret_norm": g_ret_norm, "g_mlstm_norm": g_mlstm_norm, "w_out": w_out,
            "g_final": g_final}


def reference(x, c, positions, norm_g, w_ada, b_ada, w_ff1, w_ff3, w_ff2, w_in, conv_w, conv_b,
              b_igate, b_fgate, g_ret_norm, g_mlstm_norm, w_out, g_final):
    for l in range(DEPTH):
        shift, scale, gate = ada_mod(c, w_ada[l, 0], b_ada[l, 0])
        h = rmsnorm(x, norm_g[l, 0]) * (1.0 + scale) + shift
        x = x + 0.5 * gate * swiglu(h, w_ff1[l, 0], w_ff3[l, 0], w_ff2[l, 0])

        shift, scale, gate = ada_mod(c, w_ada[l, 1], b_ada[l, 1])
        h = rmsnorm(x, norm_g[l, 1]) * (1.0 + scale) + shift
        z = h @ w_in[l]
        o = 0
        r_q, r_k, r_v, r_g = (z[..., o + j * D_RET:o + (j + 1) * D_RET] for j in range(4))
        o = 4 * D_RET
        m_qk = z[..., o:o + 2 * D_MLSTM]
        m_v = z[..., o + 2 * D_MLSTM:o + 3 * D_MLSTM]
        m_o = z[..., o + 3 * D_MLSTM:o + 4 * D_MLSTM]
        o = o + 4 * D_MLSTM
        m_i = z[..., o:o + MLSTM_HEADS] + b_igate[l]
        m_f = z[..., o + MLSTM_HEADS:o + 2 * MLSTM_HEADS] + b_fgate[l]

        rq = rotary(to_heads(r_q, RET_HEADS), positions)
        rk = rotary(to_heads(r_k, RET_HEADS), positions)
        ret = retention_chunkwise(rq, rk, to_heads(r_v, RET_HEADS))
        y_ret = jax.nn.silu(r_g.astype(jnp.float32)) * head_norm(ret, g_ret_norm[l])

        qk = jax.nn.silu(causal_depthwise_conv(m_qk, conv_w[l], conv_b[l]))
        mq = to_heads(qk[..., :D_MLSTM], MLSTM_HEADS)
        mk = to_heads(qk[..., D_MLSTM:], MLSTM_HEADS)
        i_pre = m_i.astype(jnp.float32).transpose(0, 2, 1)
        f_pre = m_f.astype(jnp.float32).transpose(0, 2, 1)
        mh = mlstm_chunkwise(mq, mk, to_heads(m_v, MLSTM_HEADS), i_pre, f_pre)
        mh = jax.nn.sigmoid(to_heads(m_o, MLSTM_HEADS)) * mh
        y_ml = head_norm(mh, g_mlstm_norm[l])

        y = jnp.concatenate([y_ret, y_ml], axis=-1).astype(x.dtype)
        x = x + gate * (y @ w_out[l])

        shift, scale, gate = ada_mod(c, w_ada[l, 2], b_ada[l, 2])
        h = rmsnorm(x, norm_g[l, 2]) * (1.0 + scale) + shift
        x = x + 0.5 * gate * swiglu(h, w_ff1[l, 1], w_ff3[l, 1], w_ff2[l, 1])
    return rmsnorm(x, g_final)
```

```python
import math
import numpy as np
import concourse.bass as bass
import concourse.mybir as mybir
from concourse.bass_utils import run_bass_kernel_spmd

F32 = mybir.dt.float32
BF16 = mybir.dt.bfloat16
I32 = mybir.dt.int32
AF = mybir.ActivationFunctionType
ALU = mybir.AluOpType
AX = mybir.AxisListType

D = 1024
DFF = 2816
SEQ = 4096
DIN = 4104
NB = 8
T = 1024
NS = T // 512
NCH = T // 128
KC = 8
NG = DFF // 256
EPS = 1e-6
LNS = math.log(128 ** -0.5)
EPOCH = 16000
CENG = ("pe", "act", "dve", "pool")
GAMMA = [1.0 - 2.0 ** (-5.0 - h) for h in range(4)]
DBG = {"mix": 99}


class Buf:
    __slots__ = ("name", "lw", "rd")

    def __init__(self, name):
        self.name = name
        self.lw = None
        self.rd = []


class Prog:
    def __init__(self, nc):
        self.nc = nc
        self.ops = {e: [] for e in CENG + ("sp",)}
        self.cnt = {e: 0 for e in CENG}
        self.dcnt = {}
        self.known = {e: {} for e in CENG + ("sp",)}
        self.nbuf = 0

    def buf(self, name=None):
        self.nbuf += 1
        return Buf(name or f"b{self.nbuf}")

    def _waits(self, eng, r, w):
        need = {}
        for b in r:
            if b.lw is not None:
                k, v = b.lw
                if need.get(k, 0) < v:
                    need[k] = v
        for b in w:
            if b.lw is not None:
                k, v = b.lw
                if need.get(k, 0) < v:
                    need[k] = v
            for k, v in b.rd:
                if need.get(k, 0) < v:
                    need[k] = v
        waits = []
        kn = self.known[eng]
        for k, v in need.items():
            if k == "pe" and eng == "pe":
                continue
            if kn.get(k, 0) >= v:
                continue
            kn[k] = v
            waits.append((k, v))
        return waits

    def _mark(self, oid, r, w):
        for b in r:
            b.rd = [x for x in b.rd if x[0] != oid[0]] + [oid]
        for b in w:
            b.lw = oid
            b.rd = []

    def op(self, eng, fn, r=(), w=()):
        waits = self._waits(eng, r, w)
        self.cnt[eng] += 1
        oid = (eng, self.cnt[eng])
        self.ops[eng].append((fn, waits, None))
        self._mark(oid, r, w)
        return oid

    def dma(self, key, fn, r=(), w=(), eng="sp"):
        waits = self._waits(eng, r, w)
        self.dcnt[key] = self.dcnt.get(key, 0) + 16
        oid = ("D:" + key, self.dcnt[key])
        self.ops[eng].append((fn, waits, key))
        self._mark(oid, r, w)
        return oid

    def emit(self):
        nc = self.nc
        sems = {}
        for e in CENG:
            n = max(1, (self.cnt[e] + EPOCH - 1) // EPOCH)
            sems[e] = [nc.alloc_semaphore(f"s_{e}{i}") for i in range(n)]
        dsem = {k: nc.alloc_semaphore("d_" + k) for k in self.dcnt}

        def do_wait(engobj, k, v):
            if k.startswith("D:"):
                engobj.wait_ge(dsem[k[2:]], v)
            else:
                engobj.wait_ge(sems[k][(v - 1) // EPOCH], (v - 1) % EPOCH + 1)

        def run(engname, engobj):
            i = 0
            for fn, waits, dkey in self.ops[engname]:
                for k, v in waits:
                    do_wait(engobj, k, v)
                ins = fn(engobj)
                if dkey is not None:
                    ins.then_inc(dsem[dkey], 16)
                else:
                    i += 1
                    ins.then_inc(sems[engname][(i - 1) // EPOCH], 1)
            if engname == "sp":
                for k, v in self.dcnt.items():
                    engobj.wait_ge(dsem[k], v)
                for e in CENG:
                    v = self.cnt[e]
                    if v:
                        engobj.wait_ge(sems[e][(v - 1) // EPOCH], (v - 1) % EPOCH + 1)

        with nc.Block() as block:
            @block.tensor
            def _(t):
                run("pe", t)

            @block.scalar
            def _(s):
                run("act", s)

            @block.vector
            def _(v):
                run("dve", v)

            @block.gpsimd
            def _(g):
                run("pool", g)

            @block.sync
            def _(sp):
                run("sp", sp)


NCON = 128 * 3 + 512 * 2 + 2
NVEC = 8 + 48 + 144 + 64 + 16 + 16 + 8 + 4


def make_consts():
    con = np.zeros((128, NCON), np.float64)
    con[:, 0:128] = np.eye(128)
    m = np.arange(128)[:, None]
    l = np.arange(128)[None, :]
    con[:, 128:256] = (m <= l)
    con[:, 256:384] = 1.0
    o = 384
    for r in range(4):
        con[:, o + r * 128:o + (r + 1) * 128] = (GAMMA[r] ** (np.arange(128) + 1.0))[None, :]
    o += 512
    for r in range(4):
        con[:, o + r * 128:o + (r + 1) * 128] = (128 ** -0.5) * (GAMMA[r] ** (-(np.arange(128) + 1.0)))[None, :]
    o += 512
    p = np.arange(128)
    invf = 10000.0 ** (-(np.arange(0, 128, 2, dtype=np.float32) / np.float32(128))).astype(np.float32)
    con[:, o] = invf[p % 64].astype(np.float64) / (2 * np.pi)
    con[:, o + 1] = np.where(p < 64, -2 * np.pi, 2 * np.pi)
    return con.astype(np.float32)


def fm(v):
    v = np.asarray(v)
    lead = v.shape[:-1]
    n = v.shape[-1] // 128
    return np.moveaxis(v.reshape(lead + (n, 128)), -1, 0).reshape(128, -1)


def make_vecs(c_b, inp):
    vec = np.zeros((128, NVEC), np.float32)
    o = 0
    vec[:, o:o + 8] = fm(c_b); o += 8
    vec[:, o:o + 48] = fm(inp["norm_g"]); o += 48
    vec[:, o:o + 144] = fm(inp["b_ada"]); o += 144
    vec[:, o:o + 64] = fm(inp["conv_w"]); o += 64
    vec[:, o:o + 16] = fm(inp["conv_b"]); o += 16
    gm = np.concatenate([inp["g_ret_norm"], inp["g_mlstm_norm"]], axis=-1)
    vec[:, o:o + 16] = fm(gm); o += 16
    vec[:, o:o + 8] = fm(inp["g_final"]); o += 8
    for l in range(2):
        vec[0:32, o + l] = np.tile(inp["b_igate"][l], 8)
        vec[0:32, o + 2 + l] = np.tile(inp["b_fgate"][l], 8)
    o += 4
    return vec


def build(nt_run=SEQ // T, stage=7):
    nc = bass.Bass("TRN2", target_bir_lowering=False)
    P = Prog(nc)
    dram = lambda n, s, d, k="ExternalInput": nc.dram_tensor(n, s, d, kind=k).ap()
    x_d = dram("x", [SEQ, D], F32)
    pos_d = dram("pos", [1, SEQ], I32)
    vec_d = dram("vecs", [128, NVEC], F32)
    con_d = dram("consts", [128, NCON], F32)
    wada_d = dram("w_ada", [2, 3, D, 3 * D], F32)
    wff1_d = dram("w_ff1", [2, 2, D, DFF], F32)
    wff3_d = dram("w_ff3", [2, 2, D, DFF], F32)
    wff2_d = dram("w_ff2", [2, 2, DFF, D], F32)
    win_d = dram("w_in", [2, D, DIN], F32)
    wout_d = dram("w_out", [2, D, D], F32)
    out_d = dram("out", [SEQ, D], F32, "ExternalOutput")

    _n = [0]

    PH = []

    def mark(name):
        PH.append((name, P.cnt["dve"]))
    P.phases = PH

    def sb(shape, dt, name=None):
        _n[0] += 1
        return nc.alloc_sbuf_tensor(name or f"t{_n[0]}", list(shape), dt).ap()

    def mm(out, lhsT, rhs, start, stop, r, w):
        P.op("pe", lambda e: e.matmul(out, lhsT, rhs, start=start, stop=stop), r, w)

    def tr(out, in_, ident, r, w):
        P.op("pe", lambda e: e.transpose(out, in_, ident), r, w)

    def act(out, in_, func, r, w, bias=None, scale=None):
        kw = {}
        if bias is not None:
            kw["bias"] = bias
        if scale is not None:
            kw["scale"] = scale
        P.op("act", lambda e: e.activation(out, in_, func, **kw), r, w)

    def tt(eng, out, a, b, op, r, w):
        P.op(eng, lambda e: e.tensor_tensor(out, a, b, op), r, w)

    def ts(eng, out, a, s1, s2, op0, op1, r, w):
        if s2 is None:
            P.op(eng, lambda e: e.tensor_scalar(out, a, s1, None, op0), r, w)
        else:
            P.op(eng, lambda e: e.tensor_scalar(out, a, s1, s2, op0, op1), r, w)

    def stt(out, a, scalar, b, op0, op1, r, w):
        P.op("dve", lambda e: e.scalar_tensor_tensor(out, a, scalar, b, op0, op1), r, w)

    def cp(eng, out, in_, r, w):
        if eng == "act":
            P.op("act", lambda e: e.activation(out, in_, AF.Copy), r, w)
        else:
            P.op(eng, lambda e: e.tensor_copy(out, in_), r, w)

    def scan(out, d0, d1, init, op0, op1, r, w):
        P.op("dve", lambda e: e.tensor_tensor_scan(out, d0, d1, init, op0, op1), r, w)

    def dma(key, out, in_, r, w):
        P.dma(key, lambda e: e.dma_start(out=out, in_=in_), r, w)

    CON = sb([128, NCON], F32, "con"); Bcon = P.buf("con")
    VEC = sb([128, NVEC], F32, "vec"); Bvec = P.buf("vec")
    dma("con", CON, con_d, [], [Bcon])
    dma("vec", VEC, vec_d, [], [Bvec])
    identf = CON[:, 0:128]; maskT = CON[:, 128:256]; onesf = CON[:, 256:384]
    wqtab = CON[:, 384:896]; wktab = CON[:, 896:1408]
    invf = CON[:, 1408:1409]; sgn = CON[:, 1409:1410]
    o = 0
    cT = VEC[:, o:o + 8]; o += 8
    normg = VEC[:, o:o + 48]; o += 48
    bada = VEC[:, o:o + 144]; o += 144
    convw = VEC[:, o:o + 64]; o += 64
    convb = VEC[:, o:o + 16]; o += 16
    gmix = VEC[:, o:o + 16]; o += 16
    gfin = VEC[:, o:o + 8]; o += 8
    bi32 = [VEC[0:32, o + l:o + l + 1] for l in range(2)]
    bf32 = [VEC[0:32, o + 2 + l:o + 3 + l] for l in range(2)]
    identb = sb([128, 128], BF16, "identb"); Bidb = P.buf("identb")
    cp("act", identb, identf, [Bcon], [Bidb])
    SM = sb([128, 64], F32, "small"); Bsm = P.buf("small")
    negbf = [SM[0:32, l:l + 1] for l in range(2)]
    for l in range(2):
        ts("dve", negbf[l], bf32[l], -1.0, None, ALU.mult, None, [Bvec], [Bsm])
    zeros32 = sb([32, 128], F32, "zeros32"); Bz32 = P.buf("z32")
    P.op("pool", lambda e: e.memset(zeros32, 0.0), [], [Bz32])

    PSB = [nc.alloc_psum_tensor(f"ps{i}", [128, 512], F32).ap() for i in range(8)]
    PB = [P.buf(f"ps{i}") for i in range(8)]
    BANKB = {b: [PB[b]] for b in range(8)}
    _ri = [0]

    def nreg():
        _ri[0] = (_ri[0] + 1) % 3
        return PSB[1 + _ri[0]], PB[1 + _ri[0]]

    _ro = [0]

    def nro():
        _ro[0] = (_ro[0] + 1) % 4
        b = (4, 5, 6, 0)[_ro[0]]
        return PSB[b], PB[b]

    from collections import deque
    _pfree = deque(range(8))

    def pget():
        b = _pfree.popleft()
        return PSB[b], PB[b], b

    def pput(b):
        _pfree.append(b)

    _pi = [0]

    def nbank4():
        _pi[0] = (_pi[0] + 1) % 3
        return PSB[_pi[0]], PB[_pi[0]]

    xT = sb([128, KC, T], F32, "xT")
    XB = [[P.buf(f"x{k}_{s}") for s in range(NS)] for k in range(KC)]
    hT = sb([128, KC, T], BF16, "hT")
    HB = [[P.buf(f"h{k}_{s}") for s in range(NS)] for k in range(KC)]
    NSC = 4
    SC = [sb([128, 512], F32, f"sc{i}") for i in range(NSC)]; BSC = [P.buf(f"sc{i}") for i in range(NSC)]
    _si = [0]

    def nsc():
        _si[0] = (_si[0] + 1) % NSC
        return SC[_si[0]], BSC[_si[0]]

    RSTD = [sb([128, 512], F32, f"rstd{i}") for i in range(2)]; BRSTD = [P.buf(f"rstd{i}") for i in range(2)]
    GT = [sb([128, 2, 512], BF16, f"gT{i}") for i in range(3)]; BGT = [P.buf(f"gT{i}") for i in range(3)]
    cosT = sb([128, T], F32, "cosT"); sinT = sb([128, T], F32, "sinT")
    Bcos = P.buf("cos"); Bsin = P.buf("sin")
    BIG = [sb([128, T + 8], F32, f"big{i}") for i in range(2)]; BBIG = [P.buf(f"big{i}") for i in range(2)]
    posi = cosT.bitcast(I32); Bposi = Bcos

    NST, NBF = 3, 6
    STG = [sb([128, 2048], F32, f"stg{i}") for i in range(NST)]; BSTG = [P.buf(f"stg{i}") for i in range(NST)]
    WBF = [sb([128, 2048], BF16, f"wbf{i}") for i in range(NBF)]; BWBF = [P.buf(f"wbf{i}") for i in range(NBF)]
    _w = {"st": 0, "bf": 0, "ce": 0}

    def wload(src, a, b):
        i = _w["st"]; _w["st"] = (i + 1) % NST
        v = STG[i][:, 0:a * b].rearrange("p (a b) -> p a b", a=a)
        dma(f"stg{i}", v, src, [], [BSTG[i]])
        return v, BSTG[i]

    def wbf(a, b):
        i = _w["bf"]; _w["bf"] = (i + 1) % NBF
        return WBF[i][:, 0:a * b].rearrange("p (a b) -> p a b", a=a), BWBF[i]

    def wcast(out, in_, r, w, scale=None):
        _w["ce"] += 1
        if scale is not None:
            act(out, in_, AF.Copy, r, w, scale=scale)
        elif _w["ce"] % 3 == 0 and not _w.get("act_only"):
            cp("pool", out, in_, r, w)
        else:
            cp("act", out, in_, r, w)

    def wpiece(src, a, b):
        sv, sbuf_ = wload(src, a, b)
        bv, bbuf = wbf(a, b)
        wcast(bv, sv, [sbuf_], [bbuf])
        return bv, bbuf

    MODS = sb([128, 6, 40], F32, "mods"); BMOD = [P.buf(f"mod{i}") for i in range(6)]
    scT = SM[:, 8:16]
    act(scT, cT, AF.Silu, [Bvec], [Bsm])
    ROWS = [sb([1, 256], F32, f"rows{i}") for i in range(1)]; BROWS = [P.buf(f"rows{i}") for i in range(1)]

    def mods_piece(idx, pi):
        l, s_ = divmod(idx, 3)
        wsrc = wada_d[l, s_].rearrange("(kc p) f -> p kc f", p=128)
        sv, sbf = wload(wsrc[:, :, pi * 256:(pi + 1) * 256], KC, 256)
        ps, pb, ib = pget()
        for kc in range(KC):
            mm(ps[0:1, 0:256], scT[:, kc:kc + 1], sv[:, kc, :], kc == 0, kc == KC - 1, [sbf, Bsm], [pb])
        rw_, brw_ = ROWS[0], BROWS[0]
        cp("dve", rw_, ps[0:1, 0:256], [pb], [brw_])
        for jj in range(2):
            mm(ps[:, 256 + jj:257 + jj], rw_[0:1, jj * 128:(jj + 1) * 128], onesf[0:1, 0:1], True, True,
               [brw_, Bcon], [pb])
        cp("dve", MODS[:, idx, pi * 2:pi * 2 + 2], ps[:, 256:258], [pb], [BMOD[idx]])
        pput(ib)

    def mods_gen(idxs):
        for idx in idxs:
            for pi in range(12):
                mods_piece(idx, pi)
                yield
            mods_finish(idx)

    def mods_finish(idx):
        s_ = idx % 3
        tt("dve", MODS[:, idx, 0:24], MODS[:, idx, 0:24], bada[:, idx * 24:(idx + 1) * 24], ALU.add,
           [BMOD[idx], Bvec], [BMOD[idx]])
        stt(MODS[:, idx, 24:32], MODS[:, idx, 8:16], 1.0, normg[:, idx * 8:(idx + 1) * 8], ALU.add, ALU.mult,
            [BMOD[idx], Bvec], [BMOD[idx]])
        ts("dve", MODS[:, idx, 32:40], MODS[:, idx, 16:24], 0.5 if s_ != 1 else 1.0, None, ALU.mult, None,
           [BMOD[idx]], [BMOD[idx]])

    for _ in mods_gen([0, 1]):
        pass

    SRET = sb([128, 2, 4, 128], F32, "sret"); SRETB = sb([128, 2, 4, 128], BF16, "sretb")
    CN = sb([128, 2, 4, 129], F32, "cn"); CNB = sb([128, 2, 4, 129], BF16, "cnb")
    BS = [[P.buf(f"S{l}{h}") for h in range(4)] for l in range(2)]
    BSb = [[P.buf(f"Sb{l}{h}") for h in range(4)] for l in range(2)]
    BC = [[P.buf(f"C{l}{h}") for h in range(4)] for l in range(2)]
    BCb = [[P.buf(f"Cb{l}{h}") for h in range(4)] for l in range(2)]
    MCAR = sb([1, 2, 4], F32, "mcar"); BMC = [P.buf(f"mc{l}") for l in range(2)]
    CCAR = sb([128, 2, 8, 4], F32, "ccar"); BCC = [[P.buf(f"cc{l}{j}") for j in range(8)] for l in range(2)]
    for l in range(2):
        for h in range(4):
            P.op("pool", lambda e, a=SRET[:, l, h, :]: e.memset(a, 0.0), [], [BS[l][h]])
            P.op("pool", lambda e, a=SRETB[:, l, h, :]: e.memset(a, 0.0), [], [BSb[l][h]])
            P.op("pool", lambda e, a=CN[:, l, h, :]: e.memset(a, 0.0), [], [BC[l][h]])
            P.op("pool", lambda e, a=CNB[:, l, h, :]: e.memset(a, 0.0), [], [BCb[l][h]])
        P.op("pool", lambda e, a=MCAR[:, l, :]: e.memset(a, 0.0), [], [BMC[l]])
        for j in range(8):
            P.op("pool", lambda e, a=CCAR[:, l, j, :]: e.memset(a, 0.0), [], [BCC[l][j]])

    VTM = sb([128, NCH, 4, 129], BF16, "vtm"); BV = [P.buf(f"v{c}") for c in range(NCH)]
    P.op("pool", lambda e: e.memset(VTM, 1.0), [], BV)
    GTM = sb([128, NCH, 4, 128], BF16, "gtm"); BG = [P.buf(f"g{c}") for c in range(NCH)]
    YT = sb([128, 8, T], BF16, "yT"); BY = [[P.buf(f"y{j}_{c}") for c in range(NCH)] for j in range(8)]
    QT = sb([128, 2, T], BF16, "qT"); KT = sb([128, 2, T], BF16, "kT")
    BQ = [[P.buf(f"q{h}{s}") for s in range(NS)] for h in range(2)]
    BK = [[P.buf(f"k{h}{s}") for s in range(NS)] for h in range(2)]
    NSM = 4
    KTM = [sb([128, 128], BF16, f"ktm{i}") for i in range(NSM)]; BKTM = [P.buf(f"ktm{i}") for i in range(NSM)]
    SCT = [sb([128, 128], BF16, f"sct{i}") for i in range(NSM)]; BSCT = [P.buf(f"sct{i}") for i in range(NSM)]
    YTM = [sb([128, 128], BF16, f"ytm{i}") for i in range(NSM)]; BYTM = [P.buf(f"ytm{i}") for i in range(NSM)]
    F1 = [sb([128, 132], F32, f"f1_{i}") for i in range(NSM)]; BF1 = [P.buf(f"f1_{i}") for i in range(NSM)]
    F2 = [sb([128, 132], F32, f"f2_{i}") for i in range(NSM)]; BF2 = [P.buf(f"f2_{i}") for i in range(NSM)]
    F3 = [sb([128, 132], F32, f"f3_{i}") for i in range(NSM)]; BF3 = [P.buf(f"f3_{i}") for i in range(NSM)]
    ST = [sb([128, 16], F32, f"st{i}") for i in range(NSM)]; BST = [P.buf(f"st{i}") for i in range(NSM)]
    _ci = [0]
    GA = [sb([32, 128], F32, f"ga{i}") for i in range(5)]; BGA = [P.buf(f"ga{i}") for i in range(5)]
    QTT = sb([32, 2, 128], F32, "qtt"); BQTT = [P.buf("qtt0"), P.buf("qtt1")]
    GS = sb([32, 16], F32, "gs"); BGS = P.buf("gs")
    ROW = sb([1, 4, 32], F32, "row"); BROW = P.buf("row")
    GSC = sb([128, 5, 32], F32, "gsc"); BGSC = P.buf("gsc")
    WCB = sb([128, 32], F32, "wcb"); BWCB = P.buf("wcb")
    WCS = sb([32, 128], F32, "wcs"); BWCS = P.buf("wcs")

    def stg_slot():
        i = _w["st"]; _w["st"] = (i + 1) % NST
        return i, STG[i][:, 0:D], BSTG[i]

    def load_x(ti):
        for blk in range(NCH):
            si, xin, bxin = stg_slot()
            dma(f"stg{si}", xin, x_d[ti * T + blk * 128: ti * T + (blk + 1) * 128, :], [], [bxin])
            for k4 in range(2):
                ps, pb = nbank4()
                for q in range(4):
                    kc = k4 * 4 + q
                    tr(ps[:, q * 128:(q + 1) * 128], xin[:, kc * 128:(kc + 1) * 128], identf,
                       [bxin, Bcon], [pb])
                dst = xT[:, k4 * 4:(k4 + 1) * 4, blk * 128:(blk + 1) * 128]
                wb = [XB[k4 * 4 + q][blk // 4] for q in range(4)]
                cp("act" if (blk + k4) % 2 else "dve", dst, ps.rearrange("p (a b) -> p a b", a=4), [pb], wb)

    def rotary_tables(ti):
        A, BA = BIG[0][:, 0:T], BBIG[0]
        Bt, BBt = BIG[1][:, 0:T], BBIG[1]
        dma("posi", posi, pos_d[0:1, ti * T:(ti + 1) * T].partition_broadcast(128), [], [Bposi])
        cp("dve", A, posi, [Bposi], [BA])
        ts("dve", A, A, invf, None, ALU.mult, None, [BA, Bcon], [BA])
        cp("dve", posi, A, [BA], [Bposi])
        cp("dve", Bt, posi, [Bposi], [BBt])
        tt("dve", A, A, Bt, ALU.subtract, [BA, BBt], [BA])
        ts("dve", Bt, A, 0.5, None, ALU.is_gt, None, [BA], [BBt])
        tt("dve", A, A, Bt, ALU.subtract, [BA, BBt], [BA])
        ts("dve", Bt, A, -0.5, None, ALU.is_lt, None, [BA], [BBt])
        tt("dve", A, A, Bt, ALU.add, [BA, BBt], [BA])
        act(sinT, A, AF.Sin, [BA, Bcon], [Bsin], scale=sgn)
        ts("dve", cosT, A, 0.25, None, ALU.add, None, [BA], [Bcos])
        ts("dve", Bt, cosT, 0.5, None, ALU.is_gt, None, [Bcos], [BBt])
        tt("dve", cosT, cosT, Bt, ALU.subtract, [Bcos, BBt], [Bcos])
        act(cosT, cosT, AF.Sin, [Bcos], [Bcos], scale=2 * math.pi)

    def rms_stats(s):
        for kc in range(KC):
            sq, bsq = nsc()
            act(sq, xT[:, kc, s * 512:(s + 1) * 512], AF.Square, [XB[kc][s]], [bsq])
            mm(PSB[7], onesf, sq, kc == 0, kc == KC - 1, [Bcon, bsq], [PB[7]])
        ln, bln = nsc()
        act(ln, PSB[7], AF.Ln, [PB[7]], [bln], bias=EPS, scale=1.0 / D)
        act(RSTD[s % 2], ln, AF.Exp, [bln], [BRSTD[s % 2]], scale=-0.5)
        return RSTD[s % 2], BRSTD[s % 2]

    def norm_mod(idx):
        for s in range(NS):
            rs, brs = rms_stats(s)
            for kc in range(KC):
                tm, btm = nsc()
                tt("dve", tm, xT[:, kc, s * 512:(s + 1) * 512], rs, ALU.mult, [XB[kc][s], brs], [btm])
                if kc % 2 == 0:
                    act(hT[:, kc, s * 512:(s + 1) * 512], tm, AF.Identity, [btm, BMOD[idx]], [HB[kc][s]],
                        bias=MODS[:, idx, kc:kc + 1], scale=MODS[:, idx, 24 + kc:25 + kc])
                else:
                    ts("pool", hT[:, kc, s * 512:(s + 1) * 512], tm, MODS[:, idx, 24 + kc:25 + kc],
                       MODS[:, idx, kc:kc + 1], ALU.mult, ALU.add, [btm, BMOD[idx]], [HB[kc][s]])

    _yi = [0]
    PRE = {}

    def ffn_pieces(l, j, g):
        w1s = wff1_d[l, j].rearrange("(kc p) f -> p kc f", p=128)
        w3s = wff3_d[l, j].rearrange("(kc p) f -> p kc f", p=128)
        _w["act_only"] = True
        W1, B1 = wpiece(w1s[:, :, g * 256:(g + 1) * 256], KC, 256)
        W3, B3 = wpiece(w3s[:, :, g * 256:(g + 1) * 256], KC, 256)
        W2, B2 = wpiece(wff2_d[l, j, g * 256:(g + 1) * 256, :].rearrange("(jj p) d -> p jj d", p=128), 2, D)
        _w["act_only"] = False
        return W1, B1, W3, B3, W2, B2

    def mixv_pieces(l):
        wsrc = win_d[l].rearrange("(kc p) f -> p kc f", p=128)
        return wpiece(wsrc[:, :, 1024:1280], KC, 256) + wpiece(wsrc[:, :, 1280:1536], KC, 256)

    def ffn(l, j, ti):
        mark(f"t{ti}.ffn{l}{j}.norm")
        idx = l * 3 + (0 if j == 0 else 2)
        nxt = None
        norm_mod(idx)
        mark(f"t{ti}.ffn{l}{j}.main")
        w1s = wff1_d[l, j].rearrange("(kc p) f -> p kc f", p=128)
        w3s = wff3_d[l, j].rearrange("(kc p) f -> p kc f", p=128)
        pend = None
        it = 0
        nxtw = PRE.pop(("ffn", l, j), None) or ffn_pieces(l, j, 0)
        for g in range(NG):
            W1, B1, W3, B3, W2, B2 = nxtw
            for s in range(NS):
                gt, bgt = GT[it % 3], BGT[it % 3]
                for jj in range(2):
                    U, bU = PSB[jj], PB[jj]
                    V, bV = PSB[2 + jj], PB[2 + jj]
                    for kc in range(KC):
                        mm(U, W1[:, kc, jj * 128:(jj + 1) * 128], hT[:, kc, s * 512:(s + 1) * 512],
                           kc == 0, kc == KC - 1, [B1, HB[kc][s]], [bU])
                    for kc in range(KC):
                        mm(V, W3[:, kc, jj * 128:(jj + 1) * 128], hT[:, kc, s * 512:(s + 1) * 512],
                           kc == 0, kc == KC - 1, [B3, HB[kc][s]], [bV])
                    su, bsu = nsc()
                    act(su, U, AF.Silu, [bU], [bsu])
                    tt("dve", gt[:, jj, :], su, V, ALU.mult, [bsu, bV], [bgt])
                    if pend is not None:
                        pend(jj)
                if s == 0:
                    if g + 1 < NG:
                        nxtw = ffn_pieces(l, j, g + 1)
                    elif j == 0:
                        PRE[("mixv", l)] = mixv_pieces(l)
                    elif l == 0:
                        PRE[("ffn", 1, 0)] = ffn_pieces(1, 0, 0)
                    elif ti + 1 < nt_run and stage >= 7:
                        PRE[("ffn", 0, 0)] = ffn_pieces(0, 0, 0)

                def ypart(half, W2=W2, B2=B2, gt=gt, bgt=bgt, s=s):
                    for dc in range(half * 4, half * 4 + 4):
                        _yi[0] = (_yi[0] + 1) % 4
                        Y, bY = PSB[4 + _yi[0]], BANKB[4 + _yi[0]]
                        for jj in range(2):
                            mm(Y, W2[:, jj, dc * 128:(dc + 1) * 128], gt[:, jj, :], jj == 0, jj == 1, [B2, bgt], bY)
                        xs = xT[:, dc, s * 512:(s + 1) * 512]
                        stt(xs, Y, MODS[:, idx, 32 + dc:33 + dc], xs, ALU.mult, ALU.add,
                            bY + [BMOD[idx], XB[dc][s]], [XB[dc][s]])
                pend = ypart
                it += 1
        pend(0)
        pend(1)

    def proj_tm(l, col0, func, dst, dstbufs):
        wsrc = win_d[l].rearrange("(kc p) f -> p kc f", p=128)
        pre = PRE.pop(("mixv", l), None) if col0 == 1024 else None
        if pre is not None:
            Wa, Ba, Wb, Bb = pre
        else:
            Wa, Ba = wpiece(wsrc[:, :, col0:col0 + 256], KC, 256)
            Wb, Bb = wpiece(wsrc[:, :, col0 + 256:col0 + 512], KC, 256)
        for c in range(NCH):
            ps, pb = nbank4()
            for half, (W, Bw) in enumerate(((Wa, Ba), (Wb, Bb))):
                for kc in range(KC):
                    mm(ps[:, half * 256:(half + 1) * 256], hT[:, kc, c * 128:(c + 1) * 128], W[:, kc, :],
                       kc == 0, kc == KC - 1, [Bw, HB[kc][c // 4]], [pb])
            act(dst[:, c, :, 0:128], ps.rearrange("p (h e) -> p h e", h=4), func, [pb], [dstbufs[c]])

    def head_norm_gen(src, bsrc, gate_ap, bgate, jh, c, k, rel=None):
        st, bst = ST[k], BST[k]
        P.op("dve", lambda e: e.bn_stats(st[:, 0:6], src), [bsrc], [bst])
        yield
        P.op("dve", lambda e: e.bn_aggr(st[:, 6:8], st[:, 0:6]), [bst], [bst])
        yield
        act(st[:, 8:9], st[:, 7:8], AF.Sqrt, [bst], [bst], bias=EPS, scale=1.0)
        yield
        P.op("dve", lambda e: e.reciprocal(st[:, 9:10], st[:, 8:9]), [bst], [bst])
        yield
        y1, by1 = F3[k], BF3[k]
        if gate_ap is not None:
            stt(y1[:, 0:128], src, st[:, 6:7], gate_ap, ALU.subtract, ALU.mult, [bsrc, bst, bgate], [by1])
            if rel is not None:
                pput(rel)
            yield
            act(YTM[k], y1[:, 0:128], AF.Copy, [by1, bst], [BYTM[k]], scale=st[:, 9:10])
        else:
            ts("dve", YTM[k], src, st[:, 6:7], st[:, 9:10], ALU.subtract, ALU.mult, [bsrc, bst], [BYTM[k]])
            if rel is not None:
                pput(rel)
        yield
        rg, brg, ig = pget()
        tr(rg.bitcast(BF16)[:, 0:128], YTM[k], identb, [BYTM[k], Bidb], [brg])
        yield
        cp("act", YT[:, jh, c * 128:(c + 1) * 128], rg.bitcast(BF16)[:, 0:128], [brg], [BY[jh][c]])
        pput(ig)

    BGR = [None]
    _bgc = [0]

    def run_chunks(make_gen, heads, SK):
        live = []

        def step(pred):
            nxt = []
            for cc, g in live:
                if pred(cc):
                    try:
                        next(g)
                    except StopIteration:
                        continue
                nxt.append((cc, g))
            live[:] = nxt
            _bgc[0] += 1
            if BGR[0] is not None and _bgc[0] % 6 == 0:
                next(BGR[0], None)
        for c in range(NCH):
            while any(cc < c - 1 for cc, _ in live):
                step(lambda cc: cc < c - 1)
            for hh in heads:
                live.append((c, make_gen(hh, c)))
            for _ in range(SK):
                step(lambda cc: True)
        while live:
            step(lambda cc: True)

    def retention(l):
        wsrc = win_d[l].rearrange("(kc p) f -> p kc f", p=128)
        proj_tm(l, 1024, AF.Copy, VTM, BV)
        proj_tm(l, 1536, AF.Silu, GTM, BG)
        if DBG["mix"] < 2:
            return
        for hp in range(2):
            WW = {}
            for nm, col0 in (("q", hp * 256), ("k", 512 + hp * 256)):
                sv, sbf = wload(wsrc[:, :, col0:col0 + 256], KC, 256)
                bn, bbn = wbf(KC, 256)
                bs_, bbs = wbf(KC, 256)
                wcast(bn, sv, [sbf], [bbn])
                s4 = sv.rearrange("p k (h t d) -> p k h t d", h=2, t=2)
                o4 = bs_.rearrange("p k (h t d) -> p k h t d", h=2, t=2)
                for hh in range(2):
                    cp("act", o4[:, :, hh, 0, :], s4[:, :, hh, 1, :], [sbf], [bbs])
                    cp("act", o4[:, :, hh, 1, :], s4[:, :, hh, 0, :], [sbf], [bbs])
                WW[nm] = (bn, bbn, bs_, bbs)
            for hh in range(2):
                r = hp * 2 + hh
                for nm, dstT, dbufs, tab in (("q", QT, BQ, wqtab), ("k", KT, BK, wktab)):
                    bn, bbn, bs_, bbs = WW[nm]
                    for s in range(NS):
                        pa, pba = nbank4()
                        pbk, pbb = nbank4()
                        for kc in range(KC):
                            mm(pa, bn[:, kc, hh * 128:(hh + 1) * 128], hT[:, kc, s * 512:(s + 1) * 512],
                               kc == 0, kc == KC - 1, [bbn, HB[kc][s]], [pba])
                        for kc in range(KC):
                            mm(pbk, bs_[:, kc, hh * 128:(hh + 1) * 128], hT[:, kc, s * 512:(s + 1) * 512],
                               kc == 0, kc == KC - 1, [bbs, HB[kc][s]], [pbb])
                        t1, bt1 = nsc()
                        t2, bt2 = nsc()
                        tt("dve", t1, pa, cosT[:, s * 512:(s + 1) * 512], ALU.mult, [pba, Bcos], [bt1])
                        tt("dve", t2, pbk, sinT[:, s * 512:(s + 1) * 512], ALU.mult, [pbb, Bsin], [bt2])
                        tt("pool", t1, t1, t2, ALU.add, [bt1, bt2], [bt1])
                        tb = tab[:, r * 128:(r + 1) * 128].unsqueeze(1).broadcast_to([128, 4, 128])
                        tt("pool", dstT[:, hh, s * 512:(s + 1) * 512].rearrange("p (c l) -> p c l", c=4),
                           t1.rearrange("p (c l) -> p c l", c=4), tb, ALU.mult, [bt1, Bcon], [dbufs[hh][s]])
            if DBG["mix"] < 3:
                continue
            def ret_gen(hh, c, hp=hp):
                r = hp * 2 + hh
                s = c // 4
                _ci[0] += 1
                k = _ci[0] % NSM
                gL = GAMMA[r] ** 128
                qc = QT[:, hh, c * 128:(c + 1) * 128]; kc_ = KT[:, hh, c * 128:(c + 1) * 128]
                S_ = SRET[:, l, r, :]; Sb_ = SRETB[:, l, r, :]
                rg, brg, ig = pget()
                tr(rg.bitcast(BF16)[:, 0:128], kc_, identb, [BK[hh][s], Bidb], [brg])
                yield
                act(KTM[k], rg.bitcast(BF16)[:, 0:128], AF.Copy, [brg], [BKTM[k]], scale=gL)
                pput(ig)
                rs_, brs, is_ = pget()
                mm(rs_[:, 0:128], kc_, qc, True, True, [BK[hh][s], BQ[hh][s]], [brs])
                yield
                tt("dve", SCT[k], rs_[:, 0:128], maskT, ALU.mult, [brs, Bcon], [BSCT[k]])
                pput(is_)
                yield
                ro, bro, io = pget()
                mm(ro[:, 0:128], SCT[k], VTM[:, c, r, 0:128], True, False, [BSCT[k], BV[c]], [bro])
                mm(ro[:, 0:128], qc, Sb_, False, True, [BQ[hh][s], BSb[l][r]], [bro])
                rk, brk, ik = pget()
                mm(rk[:, 0:128], KTM[k], VTM[:, c, r, 0:128], True, True, [BKTM[k], BV[c]], [brk])
                yield
                stt(S_, S_, gL, rk[:, 0:128], ALU.mult, ALU.add, [BS[l][r], brk], [BS[l][r]])
                pput(ik)
                yield
                cp("act", Sb_, S_, [BS[l][r]], [BSb[l][r]])
                yield from head_norm_gen(ro[:, 0:128], bro, GTM[:, c, r, :], BG[c], r, c, k, rel=io)
            mark(f"  ret.chunks{hp}")
            run_chunks(ret_gen, range(2), 7)
            mark(f"  ret.after{hp}")

    def mlstm(l):
        wsrc = win_d[l].rearrange("(kc p) f -> p kc f", p=128)
        if DBG["mix"] < 4:
            return
        proj_tm(l, 3072, AF.Copy, VTM, BV)
        proj_tm(l, 3584, AF.Sigmoid, GTM, BG)
        mark("  ml.gates")
        sv, sbf = wload(wsrc[:, :, 4096:4104], KC, 8)
        wg, bwg = wbf(KC, 8)
        wcast(wg, sv, [sbf], [bwg])
        gp, bgp = PSB[7], PB[7]
        for c in range(NCH):
            for t_ in range(2):
                for kc in range(KC):
                    mm(gp[:, t_ * 32 + c * 4:t_ * 32 + c * 4 + 4], hT[:, kc, c * 128:(c + 1) * 128],
                       wg[:, kc, t_ * 4:t_ * 4 + 4], kc == 0, kc == KC - 1, [bwg, HB[kc][c // 4]], [bgp])
        gtok, bgtok = nsc()
        cp("dve", gtok[:, 0:64], gp[:, 0:64], [bgp], [bgtok])
        ri, bri = nreg()
        rf, brf = nreg()
        tr(ri[0:32, 0:128], gtok[:, 0:32], identf, [bgtok, Bcon], [bri])
        tr(rf[0:32, 0:128], gtok[:, 32:64], identf, [bgtok, Bcon], [brf])
        e1, l1, nb, a_, cm = GA
        be1, bl1, bnb, ba, bcm = BGA
        mx, bmx = cm, bcm
        act(e1, rf[0:32, 0:128], AF.Exp, [brf, Bsm], [be1], bias=negbf[l], scale=-1.0)
        act(l1, e1, AF.Ln, [be1], [bl1], bias=1.0, scale=1.0)
        scan(nb, onesf[0:32, :], l1, 0.0, ALU.mult, ALU.add, [Bcon, bl1], [bnb])
        stt(a_, ri[0:32, 0:128], bi32[l], nb, ALU.add, ALU.add, [bri, Bvec, bnb], [ba])
        scan(cm, zeros32, a_, -1e30, ALU.add, ALU.max, [Bz32, ba], [bcm])
        tt("dve", GS[:, 0:1], cm[:, 127:128], nb[:, 127:128], ALU.subtract, [bcm, bnb], [BGS])
        ts("dve", GS[:, 1:2], nb[:, 127:128], -1.0, None, ALU.mult, None, [bnb], [BGS])
        rr, brr = nreg()
        tr(rr[0:1, 0:32], GS[:, 0:1], identf[0:32, 0:32], [BGS, Bcon], [brr])
        tr(rr[0:1, 32:64], GS[:, 1:2], identf[0:32, 0:32], [BGS, Bcon], [brr])
        cp("dve", ROW[:, 0:2, :], rr[0:1, 0:64].rearrange("p (a b) -> p a b", a=2), [brr], [BROW])
        rowv = lambda i: ROW[:, i, :].rearrange("p (c h) -> p c h", h=4)
        for h in range(4):
            scan(rowv(3)[:, :, h], rowv(1)[:, :, h], rowv(0)[:, :, h], MCAR[:, l, h:h + 1], ALU.add, ALU.max,
                 [BROW, BMC[l]], [BROW])
        cp("dve", ROW[:, 2, 4:32], ROW[:, 3, 0:28], [BROW], [BROW])
        cp("dve", ROW[:, 2, 0:4], MCAR[:, l, :], [BROW, BMC[l]], [BROW])
        cp("dve", MCAR[:, l, :], ROW[:, 3, 28:32], [BROW], [BMC[l]])
        rc, brc = nreg()
        mm(rc[0:32, 0:1], ROW[:, 2, :], onesf[0:1, 0:1], True, True, [BROW, Bcon], [brc])
        mm(rc[0:32, 1:2], ROW[:, 3, :], onesf[0:1, 0:1], True, True, [BROW, Bcon], [brc])
        cp("dve", GS[:, 2:4], rc[0:32, 0:2], [brc], [BGS])
        ts("dve", GS[:, 4:5], GS[:, 2:3], LNS, None, ALU.add, None, [BGS], [BGS])
        tt("dve", GS[:, 5:6], GS[:, 1:2], GS[:, 3:4], ALU.subtract, [BGS], [BGS])
        tt("dve", GS[:, 6:7], GS[:, 5:6], GS[:, 2:3], ALU.add, [BGS], [BGS])
        act(GS[:, 7:8], GS[:, 6:7], AF.Exp, [BGS], [BGS])
        ts("dve", mx, cm, GS[:, 2:3], None, ALU.max, None, [bcm, BGS], [bmx])
        rq, brq = nreg()

        def qexp(q, src, rbufs, **kw):
            act(QTT[:, q % 2, :], src, AF.Exp, rbufs, [BQTT[q % 2]], **kw)
            tr(rq[:, q * 32:(q + 1) * 32], QTT[:, q % 2, :], identf[0:32, 0:32], [BQTT[q % 2], Bcon], [brq])
        qexp(0, a_, [ba])
        qexp(1, mx, [bmx], bias=LNS, scale=-1.0)
        qexp(2, mx, [bmx, BGS], bias=GS[:, 4:5], scale=-1.0)
        tt("dve", e1, nb, mx, ALU.subtract, [bnb, bmx], [be1])
        qexp(3, e1, [be1])
        qexp(4, a_, [ba, BGS], bias=GS[:, 5:6], scale=1.0)
        cp("dve", GSC, rq[:, 0:160].rearrange("p (q j) -> p q j", q=5), [brq], [BGSC])
        ts("dve", WCS, onesf[0:32, :], GS[:, 7:8], None, ALU.mult, None, [Bcon, BGS], [BWCS])
        rw, brw = nreg()
        mm(rw[:, 0:32], WCS, identf[0:32, 0:32], True, True, [BWCS, Bcon], [brw])
        cp("dve", WCB, rw[:, 0:32], [brw], [BWCB])
        mark("  ml.pairs")
        if DBG["mix"] < 5:
            return
        for hp in range(2):
            WQ, BWQ = wpiece(wsrc[:, :, 2048 + hp * 256:2048 + (hp + 1) * 256], KC, 256)
            WK, BWK = wpiece(wsrc[:, :, 2560 + hp * 256:2560 + (hp + 1) * 256], KC, 256)
            for hh in range(2):
                h = hp * 2 + hh
                for j, Wp, Bp, dstT, dbufs in ((h, WQ, BWQ, QT, BQ), (4 + h, WK, BWK, KT, BK)):
                    ub, bub = BIG[j // 4], BBIG[j // 4]
                    cp("pool", ub[:, 0:3], CCAR[:, l, j, 0:3], [BCC[l][j]], [bub])
                    for s in range(NS):
                        ps, pb = nbank4()
                        for kc in range(KC):
                            mm(ps, Wp[:, kc, hh * 128:(hh + 1) * 128], hT[:, kc, s * 512:(s + 1) * 512],
                               kc == 0, kc == KC - 1, [Bp, HB[kc][s]], [pb])
                        cp("act", ub[:, 3 + s * 512:3 + (s + 1) * 512], ps, [pb], [bub])
                    cp("pool", CCAR[:, l, j, 0:3], ub[:, T:T + 3], [bub], [BCC[l][j]])
                    for s in range(NS):
                        ac, bac = nsc()
                        lo = s * 512
                        cw = lambda t, j=j: convw[:, (l * 4 + t) * 8 + j:(l * 4 + t) * 8 + j + 1]
                        ts("dve", ac, ub[:, lo:lo + 512], cw(0), None, ALU.mult, None, [bub, Bvec], [bac])
                        for t in range(1, 4):
                            stt(ac, ub[:, lo + t:lo + t + 512], cw(t), ac, ALU.mult, ALU.add, [bub, Bvec, bac], [bac])
                        act(dstT[:, hh, lo:lo + 512], ac, AF.Silu, [bac, Bvec], [dbufs[hh][s]],
                            bias=convb[:, l * 8 + j:l * 8 + j + 1], scale=1.0)
            if DBG["mix"] < 6:
                continue
            def ml_gen(hh, c, hp=hp):
                h = hp * 2 + hh
                s = c // 4
                _ci[0] += 1
                k = _ci[0] % NSM
                jc = c * 4 + h
                qc = QT[:, hh, c * 128:(c + 1) * 128]; kc_ = KT[:, hh, c * 128:(c + 1) * 128]
                C_ = CN[:, l, h, :]; Cb_ = CNB[:, l, h, :]
                f1, bf1 = F1[k], BF1[k]
                f2, bf2 = F2[k], BF2[k]
                st, bst = ST[k], BST[k]
                rg, brg, ig = pget()
                tr(rg.bitcast(BF16)[:, 0:128], kc_, identb, [BK[hh][s], Bidb], [brg])
                yield
                act(KTM[k], rg.bitcast(BF16)[:, 0:128], AF.Copy, [brg, BGSC], [BKTM[k]], scale=GSC[:, 4, jc:jc + 1])
                pput(ig)
                rs_, brs, is_ = pget()
                mm(rs_[:, 0:128], kc_, qc, True, True, [BK[hh][s], BQ[hh][s]], [brs])
                yield
                stt(SCT[k], rs_[:, 0:128], GSC[:, 0, jc:jc + 1], maskT, ALU.mult, ALU.mult, [brs, BGSC, Bcon], [BSCT[k]])
                pput(is_)
                yield
                r2, br2, i2 = pget()
                mm(r2[:, 0:129], qc, Cb_, True, True, [BQ[hh][s], BCb[l][h]], [br2])
                rk, brk, ik = pget()
                mm(rk[:, 0:129], KTM[k], VTM[:, c, h, :], True, True, [BKTM[k], BV[c]], [brk])
                yield
                act(f1[:, 0:129], r2[:, 0:129], AF.Copy, [br2, BGSC], [bf1], scale=GSC[:, 2, jc:jc + 1])
                pput(i2)
                stt(C_, C_, WCB[:, jc:jc + 1], rk[:, 0:129], ALU.mult, ALU.add, [BC[l][h], BWCB, brk], [BC[l][h]])
                pput(ik)
                r1, br1, i1 = pget()
                mm(r1[:, 0:129], SCT[k], VTM[:, c, h, :], True, True, [BSCT[k], BV[c]], [br1])
                yield
                cp("act", Cb_, C_, [BC[l][h]], [BCb[l][h]])
                stt(f2[:, 0:129], r1[:, 0:129], GSC[:, 1, jc:jc + 1], f1[:, 0:129], ALU.mult, ALU.add,
                    [br1, BGSC, bf1], [bf2])
                pput(i1)
                yield
                act(st[:, 10:11], f2[:, 128:129], AF.Abs, [bf2], [bst])
                yield
                tt("dve", st[:, 11:12], st[:, 10:11], GSC[:, 3, jc:jc + 1], ALU.max, [bst, BGSC], [bst])
                yield
                P.op("dve", lambda e, o_=st[:, 12:13], i_=st[:, 11:12]: e.reciprocal(o_, i_), [bst], [bst])
                yield
                stt(f1[:, 0:128], f2[:, 0:128], st[:, 12:13], GTM[:, c, h, :], ALU.mult, ALU.mult,
                    [bf2, bst, BG[c]], [bf1])
                yield
                yield from head_norm_gen(f1[:, 0:128], bf1, None, None, 4 + h, c, k)
            mark(f"  ml.chunks{hp}")
            run_chunks(ml_gen, range(2), 9)
            mark(f"  ml.after{hp}")

    def mixer(l, ti):
        idx = l * 3 + 1
        mark(f"t{ti}.mix{l}.norm")
        norm_mod(idx)
        mark(f"t{ti}.mix{l}.ret")
        if ti == 0:
            BGR[0] = mods_gen([2, 3, 4] if l == 0 else [5])
        retention(l)
        mark(f"t{ti}.mix{l}.mlstm")
        mlstm(l)
        if BGR[0] is not None:
            for _ in BGR[0]:
                pass
            BGR[0] = None
        mark(f"t{ti}.mix{l}.wout")
        if DBG["mix"] < 7:
            return
        WO = []
        for pj in range(4):
            sv, sbf = wload(wout_d[l, pj * 256:(pj + 1) * 256, :].rearrange("(jj p) d -> p jj d", p=128), 2, D)
            bv, bbv = wbf(2, D)
            for jj in range(2):
                jidx = l * 8 + pj * 2 + jj
                act(bv[:, jj, :], sv[:, jj, :], AF.Copy, [sbf, Bvec], [bbv], scale=gmix[:, jidx:jidx + 1])
            WO.append((bv, bbv))
        for s in range(NS):
            for dc in range(KC):
                Y, bY = nbank4()
                for j in range(8):
                    bv, bbv = WO[j // 2]
                    mm(Y, bv[:, j % 2, dc * 128:(dc + 1) * 128], YT[:, j, s * 512:(s + 1) * 512], j == 0, j == 7,
                       [bbv] + [BY[j][c] for c in range(s * 4, s * 4 + 4)], [bY])
                xs = xT[:, dc, s * 512:(s + 1) * 512]
                stt(xs, Y, MODS[:, idx, 32 + dc:33 + dc], xs, ALU.mult, ALU.add, [bY, BMOD[idx], XB[dc][s]], [XB[dc][s]])
        PRE[("ffn", l, 1)] = ffn_pieces(l, 1, 0)

    def store_out(ti, final):
        rsl = []
        for s in range(NS):
            if final:
                rs, brs = rms_stats(s)
            for kc in range(KC):
                xs = xT[:, kc, s * 512:(s + 1) * 512]
                if final:
                    stt(xs, xs, gfin[:, kc:kc + 1], rs, ALU.mult, ALU.mult, [XB[kc][s], Bvec, brs], [XB[kc][s]])
        for blk in range(NCH):
            s = blk // 4
            si, xo, bxo = stg_slot()
            for k4 in range(2):
                ps, pb = nbank4()
                for q in range(4):
                    kc = k4 * 4 + q
                    tr(ps[:, q * 128:(q + 1) * 128], xT[:, kc, blk * 128:(blk + 1) * 128], identf,
                       [XB[kc][s], Bcon], [pb])
                cp("act" if k4 else "dve", xo[:, k4 * 512:(k4 + 1) * 512], ps, [pb], [bxo])
            dma(f"stg{si}", out_d[ti * T + blk * 128: ti * T + (blk + 1) * 128, :], xo, [bxo], [])

    for ti in range(nt_run):
        mark(f"t{ti}.load")
        load_x(ti)
        rotary_tables(ti)
        st = 0
        for l in range(2):
            for sub in range(3):
                st += 1
                if st > stage:
                    break
                if sub == 0:
                    ffn(l, 0, ti)
                elif sub == 1:
                    mixer(l, ti)
                else:
                    ffn(l, 1, ti)
        mark(f"t{ti}.store")
        store_out(ti, final=(stage >= 7))
    mark("end")
    P.emit()
    return nc, P


_CACHE = {}


def kernel(**inputs):
    inp = {k: np.asarray(v) for k, v in inputs.items()}
    if "nc" not in _CACHE:
        _CACHE["nc"] = build()[0]
    nc = _CACHE["nc"]
    con = make_consts()
    shared = {k: np.ascontiguousarray(inp[k], dtype=np.float32)
              for k in ("w_ada", "w_ff1", "w_ff3", "w_ff2", "w_in", "w_out")}
    in_maps = []
    for b in range(NB):
        m = dict(shared)
        m["x"] = np.ascontiguousarray(inp["x"][b], dtype=np.float32)
        m["pos"] = np.ascontiguousarray(inp["positions"][b].reshape(1, SEQ), dtype=np.int32)
        m["vecs"] = make_vecs(inp["c"][b], inp)
        m["consts"] = con
        in_maps.append(m)
    res = run_bass_kernel_spmd(nc, in_maps, core_ids=list(range(NB)))
    return np.stack([np.asarray(res.results[b]["out"], dtype=np.float32) for b in range(NB)], axis=0)
```

```python
import math
import numpy as np
import concourse.bass as bass
import concourse.mybir as mybir
from concourse.bass_utils import run_bass_kernel_spmd

F32 = mybir.dt.float32
BF16 = mybir.dt.bfloat16
I32 = mybir.dt.int32
AF = mybir.ActivationFunctionType
ALU = mybir.AluOpType
AX = mybir.AxisListType

D = 1024
DFF = 2816
SEQ = 4096
DIN = 4104
NB = 8
T = 1024
NS = T // 512
NCH = T // 128
KC = 8
NG = DFF // 256
EPS = 1e-6
LNS = math.log(128 ** -0.5)
EPOCH = 16000
CENG = ("pe", "act", "dve", "pool")
GAMMA = [1.0 - 2.0 ** (-5.0 - h) for h in range(4)]
DBG = {"mix": 99}


class Buf:
    __slots__ = ("name", "lw", "rd")

    def __init__(self, name):
        self.name = name
        self.lw = None
        self.rd = []


class Prog:
    def __init__(self, nc):
        self.nc = nc
        self.ops = {e: [] for e in CENG + ("sp",)}
        self.cnt = {e: 0 for e in CENG}
        self.dcnt = {}
        self.known = {e: {} for e in CENG + ("sp",)}
        self.nbuf = 0

    def buf(self, name=None):
        self.nbuf += 1
        return Buf(name or f"b{self.nbuf}")

    def _waits(self, eng, r, w):
        need = {}
        for b in r:
            if b.lw is not None:
                k, v = b.lw
                if need.get(k, 0) < v:
                    need[k] = v
        for b in w:
            if b.lw is not None:
                k, v = b.lw
                if need.get(k, 0) < v:
                    need[k] = v
            for k, v in b.rd:
                if need.get(k, 0) < v:
                    need[k] = v
        waits = []
        kn = self.known[eng]
        for k, v in need.items():
            if k == "pe" and eng == "pe":
                continue
            if kn.get(k, 0) >= v:
                continue
            kn[k] = v
            waits.append((k, v))
        return waits

    def _mark(self, oid, r, w):
        for b in r:
            b.rd = [x for x in b.rd if x[0] != oid[0]] + [oid]
        for b in w:
            b.lw = oid
            b.rd = []

    def op(self, eng, fn, r=(), w=()):
        waits = self._waits(eng, r, w)
        self.cnt[eng] += 1
        oid = (eng, self.cnt[eng])
        self.ops[eng].append((fn, waits, None))
        self._mark(oid, r, w)
        return oid

    def dma(self, key, fn, r=(), w=(), eng="sp"):
        waits = self._waits(eng, r, w)
        self.dcnt[key] = self.dcnt.get(key, 0) + 16
        oid = ("D:" + key, self.dcnt[key])
        self.ops[eng].append((fn, waits, key))
        self._mark(oid, r, w)
        return oid

    def emit(self):
        nc = self.nc
        waited = {e: set() for e in CENG}
        for e in self.ops:
            for fn, waits, dkey in self.ops[e]:
                for k, v in waits:
                    if not k.startswith("D:"):
                        waited[k].add(v)
        for e in CENG:
            if self.cnt[e]:
                waited[e].add(self.cnt[e])
        rank = {e: {idx: i + 1 for i, idx in enumerate(sorted(waited[e]))} for e in CENG}
        sems = {}
        for e in CENG:
            n = max(1, (len(rank[e]) + EPOCH - 1) // EPOCH)
            sems[e] = [nc.alloc_semaphore(f"s_{e}{i}") for i in range(n)]
        dsem = {k: nc.alloc_semaphore("d_" + k) for k in self.dcnt}

        def do_wait(engobj, k, v):
            if k.startswith("D:"):
                engobj.wait_ge(dsem[k[2:]], v)
            else:
                r = rank[k][v]
                engobj.wait_ge(sems[k][(r - 1) // EPOCH], (r - 1) % EPOCH + 1)

        def run(engname, engobj):
            i = 0
            for fn, waits, dkey in self.ops[engname]:
                for k, v in waits:
                    do_wait(engobj, k, v)
                ins = fn(engobj)
                if dkey is not None:
                    ins.then_inc(dsem[dkey], 16)
                else:
                    i += 1
                    r = rank[engname].get(i)
                    if r is not None:
                        ins.then_inc(sems[engname][(r - 1) // EPOCH], 1)
            if engname == "sp":
                for k, v in self.dcnt.items():
                    engobj.wait_ge(dsem[k], v)
                for e in CENG:
                    v = self.cnt[e]
                    if v:
                        do_wait(engobj, e, v)

        with nc.Block() as block:
            @block.tensor
            def _(t):
                run("pe", t)

            @block.scalar
            def _(s):
                run("act", s)

            @block.vector
            def _(v):
                run("dve", v)

            @block.gpsimd
            def _(g):
                run("pool", g)

            @block.sync
            def _(sp):
                run("sp", sp)


NCON = 128 * 3 + 512 * 2 + 2
NVEC = 8 + 48 + 144 + 64 + 16 + 16 + 8 + 4


def make_consts():
    con = np.zeros((128, NCON), np.float64)
    con[:, 0:128] = np.eye(128)
    m = np.arange(128)[:, None]
    l = np.arange(128)[None, :]
    con[:, 128:256] = (m <= l)
    con[:, 256:384] = 1.0
    o = 384
    for r in range(4):
        con[:, o + r * 128:o + (r + 1) * 128] = (GAMMA[r] ** (np.arange(128) + 1.0))[None, :]
    o += 512
    for r in range(4):
        con[:, o + r * 128:o + (r + 1) * 128] = (128 ** -0.5) * (GAMMA[r] ** (-(np.arange(128) + 1.0)))[None, :]
    o += 512
    p = np.arange(128)
    invf = 10000.0 ** (-(np.arange(0, 128, 2, dtype=np.float32) / np.float32(128))).astype(np.float32)
    con[:, o] = invf[p % 64].astype(np.float64) / (2 * np.pi)
    con[:, o + 1] = np.where(p < 64, -2 * np.pi, 2 * np.pi)
    return con.astype(np.float32)


def fm(v):
    v = np.asarray(v)
    lead = v.shape[:-1]
    n = v.shape[-1] // 128
    return np.moveaxis(v.reshape(lead + (n, 128)), -1, 0).reshape(128, -1)


def make_vecs(c_b, inp):
    vec = np.zeros((128, NVEC), np.float32)
    o = 0
    vec[:, o:o + 8] = fm(c_b); o += 8
    vec[:, o:o + 48] = fm(inp["norm_g"]); o += 48
    vec[:, o:o + 144] = fm(inp["b_ada"]); o += 144
    vec[:, o:o + 64] = fm(inp["conv_w"]); o += 64
    vec[:, o:o + 16] = fm(inp["conv_b"]); o += 16
    gm = np.concatenate([inp["g_ret_norm"], inp["g_mlstm_norm"]], axis=-1)
    vec[:, o:o + 16] = fm(gm); o += 16
    vec[:, o:o + 8] = fm(inp["g_final"]); o += 8
    for l in range(2):
        vec[0:32, o + l] = np.tile(inp["b_igate"][l], 8)
        vec[0:32, o + 2 + l] = np.tile(inp["b_fgate"][l], 8)
    o += 4
    return vec


def build(nt_run=SEQ // T, stage=7):
    nc = bass.Bass("TRN2", target_bir_lowering=False)
    P = Prog(nc)
    dram = lambda n, s, d, k="ExternalInput": nc.dram_tensor(n, s, d, kind=k).ap()
    x_d = dram("x", [SEQ, D], F32)
    pos_d = dram("pos", [1, SEQ], I32)
    vec_d = dram("vecs", [128, NVEC], F32)
    con_d = dram("consts", [128, NCON], F32)
    wada_d = dram("w_ada", [2, 3, D, 3 * D], F32)
    wff1_d = dram("w_ff1", [2, 2, D, DFF], F32)
    wff3_d = dram("w_ff3", [2, 2, D, DFF], F32)
    wff2_d = dram("w_ff2", [2, 2, DFF, D], F32)
    win_d = dram("w_in", [2, D, DIN], F32)
    wout_d = dram("w_out", [2, D, D], F32)
    out_d = dram("out", [SEQ, D], F32, "ExternalOutput")

    _n = [0]

    PH = []

    def mark(name):
        PH.append((name, P.cnt["dve"]))
    P.phases = PH

    def sb(shape, dt, name=None):
        _n[0] += 1
        return nc.alloc_sbuf_tensor(name or f"t{_n[0]}", list(shape), dt).ap()

    def mm(out, lhsT, rhs, start, stop, r, w):
        P.op("pe", lambda e: e.matmul(out, lhsT, rhs, start=start, stop=stop), r, w)

    def tr(out, in_, ident, r, w):
        P.op("pe", lambda e: e.transpose(out, in_, ident), r, w)

    def act(out, in_, func, r, w, bias=None, scale=None):
        kw = {}
        if bias is not None:
            kw["bias"] = bias
        if scale is not None:
            kw["scale"] = scale
        P.op("act", lambda e: e.activation(out, in_, func, **kw), r, w)

    def tt(eng, out, a, b, op, r, w):
        P.op(eng, lambda e: e.tensor_tensor(out, a, b, op), r, w)

    def ts(eng, out, a, s1, s2, op0, op1, r, w):
        if s2 is None:
            P.op(eng, lambda e: e.tensor_scalar(out, a, s1, None, op0), r, w)
        else:
            P.op(eng, lambda e: e.tensor_scalar(out, a, s1, s2, op0, op1), r, w)

    def stt(out, a, scalar, b, op0, op1, r, w):
        P.op("dve", lambda e: e.scalar_tensor_tensor(out, a, scalar, b, op0, op1), r, w)

    def cp(eng, out, in_, r, w):
        if eng == "act":
            P.op("act", lambda e: e.activation(out, in_, AF.Copy), r, w)
        else:
            P.op(eng, lambda e: e.tensor_copy(out, in_), r, w)

    def scan(out, d0, d1, init, op0, op1, r, w):
        P.op("dve", lambda e: e.tensor_tensor_scan(out, d0, d1, init, op0, op1), r, w)

    def dma(key, out, in_, r, w):
        P.dma(key, lambda e: e.dma_start(out=out, in_=in_), r, w)

    CON = sb([128, NCON], F32, "con"); Bcon = P.buf("con")
    VEC = sb([128, NVEC], F32, "vec"); Bvec = P.buf("vec")
    dma("con", CON, con_d, [], [Bcon])
    dma("vec", VEC, vec_d, [], [Bvec])
    identf = CON[:, 0:128]; maskT = CON[:, 128:256]; onesf = CON[:, 256:384]
    wqtab = CON[:, 384:896]; wktab = CON[:, 896:1408]
    invf = CON[:, 1408:1409]; sgn = CON[:, 1409:1410]
    o = 0
    cT = VEC[:, o:o + 8]; o += 8
    normg = VEC[:, o:o + 48]; o += 48
    bada = VEC[:, o:o + 144]; o += 144
    convw = VEC[:, o:o + 64]; o += 64
    convb = VEC[:, o:o + 16]; o += 16
    gmix = VEC[:, o:o + 16]; o += 16
    gfin = VEC[:, o:o + 8]; o += 8
    bi32 = [VEC[0:32, o + l:o + l + 1] for l in range(2)]
    bf32 = [VEC[0:32, o + 2 + l:o + 3 + l] for l in range(2)]
    identb = sb([128, 128], BF16, "identb"); Bidb = P.buf("identb")
    cp("act", identb, identf, [Bcon], [Bidb])
    SM = sb([128, 64], F32, "small"); Bsm = P.buf("small")
    negbf = [SM[0:32, l:l + 1] for l in range(2)]
    for l in range(2):
        ts("dve", negbf[l], bf32[l], -1.0, None, ALU.mult, None, [Bvec], [Bsm])
    zeros32 = sb([32, 128], F32, "zeros32"); Bz32 = P.buf("z32")
    P.op("pool", lambda e: e.memset(zeros32, 0.0), [], [Bz32])

    PSB = [nc.alloc_psum_tensor(f"ps{i}", [128, 512], F32).ap() for i in range(8)]
    PB = [P.buf(f"ps{i}") for i in range(8)]
    BANKB = {b: [PB[b]] for b in range(8)}
    _ri = [0]

    def nreg():
        _ri[0] = (_ri[0] + 1) % 3
        return PSB[1 + _ri[0]], PB[1 + _ri[0]]

    _ro = [0]

    def nro():
        _ro[0] = (_ro[0] + 1) % 4
        b = (4, 5, 6, 0)[_ro[0]]
        return PSB[b], PB[b]

    from collections import deque
    _pfree = deque(range(8))

    def pget():
        b = _pfree.popleft()
        return PSB[b], PB[b], b

    def pput(b):
        _pfree.append(b)

    _pi = [0]

    def nbank4():
        _pi[0] = (_pi[0] + 1) % 3
        return PSB[_pi[0]], PB[_pi[0]]

    xT = sb([128, KC, T], F32, "xT")
    XB = [[P.buf(f"x{k}_{s}") for s in range(NS)] for k in range(KC)]
    hT = sb([128, KC, T], BF16, "hT")
    HB = [[P.buf(f"h{k}_{s}") for s in range(NS)] for k in range(KC)]
    NSC = 4
    SC = [sb([128, 512], F32, f"sc{i}") for i in range(NSC)]; BSC = [P.buf(f"sc{i}") for i in range(NSC)]
    _si = [0]

    def nsc():
        _si[0] = (_si[0] + 1) % NSC
        return SC[_si[0]], BSC[_si[0]]

    RSTD = [sb([128, 512], F32, f"rstd{i}") for i in range(2)]; BRSTD = [P.buf(f"rstd{i}") for i in range(2)]
    GT = [sb([128, 2, 512], BF16, f"gT{i}") for i in range(3)]; BGT = [P.buf(f"gT{i}") for i in range(3)]
    cosT = sb([128, T], F32, "cosT"); sinT = sb([128, T], F32, "sinT")
    Bcos = P.buf("cos"); Bsin = P.buf("sin")
    BIG = [sb([128, T + 8], F32, f"big{i}") for i in range(2)]; BBIG = [P.buf(f"big{i}") for i in range(2)]
    posi = cosT.bitcast(I32); Bposi = Bcos

    NST, NBF = 3, 6
    STG = [sb([128, 2048], F32, f"stg{i}") for i in range(NST)]; BSTG = [P.buf(f"stg{i}") for i in range(NST)]
    WBF = [sb([128, 2048], BF16, f"wbf{i}") for i in range(NBF)]; BWBF = [P.buf(f"wbf{i}") for i in range(NBF)]
    _w = {"st": 0, "bf": 0, "ce": 0}

    def wload(src, a, b):
        i = _w["st"]; _w["st"] = (i + 1) % NST
        v = STG[i][:, 0:a * b].rearrange("p (a b) -> p a b", a=a)
        dma(f"stg{i}", v, src, [], [BSTG[i]])
        return v, BSTG[i]

    def wbf(a, b):
        i = _w["bf"]; _w["bf"] = (i + 1) % NBF
        return WBF[i][:, 0:a * b].rearrange("p (a b) -> p a b", a=a), BWBF[i]

    def wcast(out, in_, r, w, scale=None):
        _w["ce"] += 1
        if scale is not None:
            act(out, in_, AF.Copy, r, w, scale=scale)
        elif _w["ce"] % 3 == 0:
            cp("pool", out, in_, r, w)
        else:
            cp("act", out, in_, r, w)

    def wpiece(src, a, b):
        sv, sbuf_ = wload(src, a, b)
        bv, bbuf = wbf(a, b)
        wcast(bv, sv, [sbuf_], [bbuf])
        return bv, bbuf

    MODS = sb([128, 6, 40], F32, "mods"); BMOD = [P.buf(f"mod{i}") for i in range(6)]
    scT = SM[:, 8:16]
    act(scT, cT, AF.Silu, [Bvec], [Bsm])
    ROWS = [sb([1, 256], F32, f"rows{i}") for i in range(1)]; BROWS = [P.buf(f"rows{i}") for i in range(1)]

    def mods_piece(idx, pi):
        l, s_ = divmod(idx, 3)
        wsrc = wada_d[l, s_].rearrange("(kc p) f -> p kc f", p=128)
        sv, sbf = wload(wsrc[:, :, pi * 256:(pi + 1) * 256], KC, 256)
        ps, pb, ib = pget()
        for kc in range(KC):
            mm(ps[0:1, 0:256], scT[:, kc:kc + 1], sv[:, kc, :], kc == 0, kc == KC - 1, [sbf, Bsm], [pb])
        rw_, brw_ = ROWS[0], BROWS[0]
        cp("dve", rw_, ps[0:1, 0:256], [pb], [brw_])
        for jj in range(2):
            mm(ps[:, 256 + jj:257 + jj], rw_[0:1, jj * 128:(jj + 1) * 128], onesf[0:1, 0:1], True, True,
               [brw_, Bcon], [pb])
        cp("dve", MODS[:, idx, pi * 2:pi * 2 + 2], ps[:, 256:258], [pb], [BMOD[idx]])
        pput(ib)

    def mods_gen(idxs):
        for idx in idxs:
            for pi in range(12):
                mods_piece(idx, pi)
                yield
            mods_finish(idx)

    def mods_finish(idx):
        s_ = idx % 3
        tt("dve", MODS[:, idx, 0:24], MODS[:, idx, 0:24], bada[:, idx * 24:(idx + 1) * 24], ALU.add,
           [BMOD[idx], Bvec], [BMOD[idx]])
        stt(MODS[:, idx, 24:32], MODS[:, idx, 8:16], 1.0, normg[:, idx * 8:(idx + 1) * 8], ALU.add, ALU.mult,
            [BMOD[idx], Bvec], [BMOD[idx]])
        ts("dve", MODS[:, idx, 32:40], MODS[:, idx, 16:24], 0.5 if s_ != 1 else 1.0, None, ALU.mult, None,
           [BMOD[idx]], [BMOD[idx]])

    for _ in mods_gen([0, 1]):
        pass

    SRET = sb([128, 2, 4, 128], F32, "sret"); SRETB = sb([128, 2, 4, 128], BF16, "sretb")
    CN = sb([128, 2, 4, 129], F32, "cn"); CNB = sb([128, 2, 4, 129], BF16, "cnb")
    BS = [[P.buf(f"S{l}{h}") for h in range(4)] for l in range(2)]
    BSb = [[P.buf(f"Sb{l}{h}") for h in range(4)] for l in range(2)]
    BC = [[P.buf(f"C{l}{h}") for h in range(4)] for l in range(2)]
    BCb = [[P.buf(f"Cb{l}{h}") for h in range(4)] for l in range(2)]
    MCAR = sb([1, 2, 4], F32, "mcar"); BMC = [P.buf(f"mc{l}") for l in range(2)]
    CCAR = sb([128, 2, 8, 4], F32, "ccar"); BCC = [[P.buf(f"cc{l}{j}") for j in range(8)] for l in range(2)]
    for l in range(2):
        for h in range(4):
            P.op("pool", lambda e, a=SRET[:, l, h, :]: e.memset(a, 0.0), [], [BS[l][h]])
            P.op("pool", lambda e, a=SRETB[:, l, h, :]: e.memset(a, 0.0), [], [BSb[l][h]])
            P.op("pool", lambda e, a=CN[:, l, h, :]: e.memset(a, 0.0), [], [BC[l][h]])
            P.op("pool", lambda e, a=CNB[:, l, h, :]: e.memset(a, 0.0), [], [BCb[l][h]])
        P.op("pool", lambda e, a=MCAR[:, l, :]: e.memset(a, 0.0), [], [BMC[l]])
        for j in range(8):
            P.op("pool", lambda e, a=CCAR[:, l, j, :]: e.memset(a, 0.0), [], [BCC[l][j]])

    VTM = sb([128, NCH, 4, 129], BF16, "vtm"); BV = [P.buf(f"v{c}") for c in range(NCH)]
    P.op("pool", lambda e: e.memset(VTM, 1.0), [], BV)
    GTM = sb([128, NCH, 4, 128], BF16, "gtm"); BG = [P.buf(f"g{c}") for c in range(NCH)]
    YT = sb([128, 8, T], BF16, "yT"); BY = [[P.buf(f"y{j}_{c}") for c in range(NCH)] for j in range(8)]
    QT = sb([128, 2, T], BF16, "qT"); KT = sb([128, 2, T], BF16, "kT")
    BQ = [[P.buf(f"q{h}{s}") for s in range(NS)] for h in range(2)]
    BK = [[P.buf(f"k{h}{s}") for s in range(NS)] for h in range(2)]
    NSM = 4
    KTM = [sb([128, 128], BF16, f"ktm{i}") for i in range(NSM)]; BKTM = [P.buf(f"ktm{i}") for i in range(NSM)]
    SCT = [sb([128, 128], BF16, f"sct{i}") for i in range(NSM)]; BSCT = [P.buf(f"sct{i}") for i in range(NSM)]
    YTM = [sb([128, 128], BF16, f"ytm{i}") for i in range(NSM)]; BYTM = [P.buf(f"ytm{i}") for i in range(NSM)]
    F1 = [sb([128, 132], F32, f"f1_{i}") for i in range(NSM)]; BF1 = [P.buf(f"f1_{i}") for i in range(NSM)]
    F2 = [sb([128, 132], F32, f"f2_{i}") for i in range(NSM)]; BF2 = [P.buf(f"f2_{i}") for i in range(NSM)]
    F3 = [sb([128, 132], F32, f"f3_{i}") for i in range(NSM)]; BF3 = [P.buf(f"f3_{i}") for i in range(NSM)]
    ST = [sb([128, 16], F32, f"st{i}") for i in range(NSM)]; BST = [P.buf(f"st{i}") for i in range(NSM)]
    _ci = [0]
    GA = [sb([32, 128], F32, f"ga{i}") for i in range(5)]; BGA = [P.buf(f"ga{i}") for i in range(5)]
    QTT = sb([32, 2, 128], F32, "qtt"); BQTT = [P.buf("qtt0"), P.buf("qtt1")]
    GS = sb([32, 16], F32, "gs"); BGS = P.buf("gs")
    ROW = sb([1, 4, 32], F32, "row"); BROW = P.buf("row")
    GSC = sb([128, 5, 32], F32, "gsc"); BGSC = P.buf("gsc")
    WCB = sb([128, 32], F32, "wcb"); BWCB = P.buf("wcb")
    WCS = sb([32, 128], F32, "wcs"); BWCS = P.buf("wcs")

    def stg_slot():
        i = _w["st"]; _w["st"] = (i + 1) % NST
        return i, STG[i][:, 0:D], BSTG[i]

    def load_x(ti):
        for blk in range(NCH):
            si, xin, bxin = stg_slot()
            dma(f"stg{si}", xin, x_d[ti * T + blk * 128: ti * T + (blk + 1) * 128, :], [], [bxin])
            for k4 in range(2):
                ps, pb = nbank4()
                for q in range(4):
                    kc = k4 * 4 + q
                    tr(ps[:, q * 128:(q + 1) * 128], xin[:, kc * 128:(kc + 1) * 128], identf,
                       [bxin, Bcon], [pb])
                dst = xT[:, k4 * 4:(k4 + 1) * 4, blk * 128:(blk + 1) * 128]
                wb = [XB[k4 * 4 + q][blk // 4] for q in range(4)]
                cp("act" if (blk + k4) % 2 else "dve", dst, ps.rearrange("p (a b) -> p a b", a=4), [pb], wb)

    def rotary_tables(ti):
        A, BA = BIG[0][:, 0:T], BBIG[0]
        Bt, BBt = BIG[1][:, 0:T], BBIG[1]
        dma("posi", posi, pos_d[0:1, ti * T:(ti + 1) * T].partition_broadcast(128), [], [Bposi])
        cp("dve", A, posi, [Bposi], [BA])
        ts("dve", A, A, invf, None, ALU.mult, None, [BA, Bcon], [BA])
        cp("dve", posi, A, [BA], [Bposi])
        cp("dve", Bt, posi, [Bposi], [BBt])
        tt("dve", A, A, Bt, ALU.subtract, [BA, BBt], [BA])
        ts("dve", Bt, A, 0.5, None, ALU.is_gt, None, [BA], [BBt])
        tt("dve", A, A, Bt, ALU.subtract, [BA, BBt], [BA])
        ts("dve", Bt, A, -0.5, None, ALU.is_lt, None, [BA], [BBt])
        tt("dve", A, A, Bt, ALU.add, [BA, BBt], [BA])
        act(sinT, A, AF.Sin, [BA, Bcon], [Bsin], scale=sgn)
        ts("dve", cosT, A, 0.25, None, ALU.add, None, [BA], [Bcos])
        ts("dve", Bt, cosT, 0.5, None, ALU.is_gt, None, [Bcos], [BBt])
        tt("dve", cosT, cosT, Bt, ALU.subtract, [Bcos, BBt], [Bcos])
        act(cosT, cosT, AF.Sin, [Bcos], [Bcos], scale=2 * math.pi)

    def rms_stats(s):
        for kc in range(KC):
            sq, bsq = nsc()
            act(sq, xT[:, kc, s * 512:(s + 1) * 512], AF.Square, [XB[kc][s]], [bsq])
            mm(PSB[7], onesf, sq, kc == 0, kc == KC - 1, [Bcon, bsq], [PB[7]])
        ln, bln = nsc()
        act(ln, PSB[7], AF.Ln, [PB[7]], [bln], bias=EPS, scale=1.0 / D)
        act(RSTD[s % 2], ln, AF.Exp, [bln], [BRSTD[s % 2]], scale=-0.5)
        return RSTD[s % 2], BRSTD[s % 2]

    def norm_mod(idx):
        for s in range(NS):
            rs, brs = rms_stats(s)
            for kc in range(KC):
                tm, btm = nsc()
                tt("dve", tm, xT[:, kc, s * 512:(s + 1) * 512], rs, ALU.mult, [XB[kc][s], brs], [btm])
                if kc % 2 == 0:
                    act(hT[:, kc, s * 512:(s + 1) * 512], tm, AF.Identity, [btm, BMOD[idx]], [HB[kc][s]],
                        bias=MODS[:, idx, kc:kc + 1], scale=MODS[:, idx, 24 + kc:25 + kc])
                else:
                    ts("pool", hT[:, kc, s * 512:(s + 1) * 512], tm, MODS[:, idx, 24 + kc:25 + kc],
                       MODS[:, idx, kc:kc + 1], ALU.mult, ALU.add, [btm, BMOD[idx]], [HB[kc][s]])

    _yi = [0]
    PRE = {}

    def ffn_pieces(l, j, g):
        w1s = wff1_d[l, j].rearrange("(kc p) f -> p kc f", p=128)
        w3s = wff3_d[l, j].rearrange("(kc p) f -> p kc f", p=128)
        W1, B1 = wpiece(w1s[:, :, g * 256:(g + 1) * 256], KC, 256)
        W3, B3 = wpiece(w3s[:, :, g * 256:(g + 1) * 256], KC, 256)
        W2, B2 = wpiece(wff2_d[l, j, g * 256:(g + 1) * 256, :].rearrange("(jj p) d -> p jj d", p=128), 2, D)
        return W1, B1, W3, B3, W2, B2

    def mixv_pieces(l):
        wsrc = win_d[l].rearrange("(kc p) f -> p kc f", p=128)
        return wpiece(wsrc[:, :, 1024:1280], KC, 256) + wpiece(wsrc[:, :, 1280:1536], KC, 256)

    def ffn(l, j, ti):
        mark(f"t{ti}.ffn{l}{j}.norm")
        idx = l * 3 + (0 if j == 0 else 2)
        nxt = None
        norm_mod(idx)
        mark(f"t{ti}.ffn{l}{j}.main")
        w1s = wff1_d[l, j].rearrange("(kc p) f -> p kc f", p=128)
        w3s = wff3_d[l, j].rearrange("(kc p) f -> p kc f", p=128)
        pend = None
        it = 0
        nxtw = PRE.pop(("ffn", l, j), None) or ffn_pieces(l, j, 0)
        for g in range(NG):
            W1, B1, W3, B3, W2, B2 = nxtw
            for s in range(NS):
                gt, bgt = GT[it % 3], BGT[it % 3]
                for jj in range(2):
                    U, bU = PSB[jj], PB[jj]
                    V, bV = PSB[2 + jj], PB[2 + jj]
                    for kc in range(KC):
                        mm(U, W1[:, kc, jj * 128:(jj + 1) * 128], hT[:, kc, s * 512:(s + 1) * 512],
                           kc == 0, kc == KC - 1, [B1, HB[kc][s]], [bU])
                    for kc in range(KC):
                        mm(V, W3[:, kc, jj * 128:(jj + 1) * 128], hT[:, kc, s * 512:(s + 1) * 512],
                           kc == 0, kc == KC - 1, [B3, HB[kc][s]], [bV])
                    su, bsu = nsc()
                    act(su, U, AF.Silu, [bU], [bsu])
                    tt("dve", gt[:, jj, :], su, V, ALU.mult, [bsu, bV], [bgt])
                    if pend is not None:
                        pend(jj)
                if s == 0:
                    if g + 1 < NG:
                        nxtw = ffn_pieces(l, j, g + 1)
                    elif j == 0:
                        PRE[("mixv", l)] = mixv_pieces(l)
                    elif l == 0:
                        PRE[("ffn", 1, 0)] = ffn_pieces(1, 0, 0)
                    elif ti + 1 < nt_run and stage >= 7:
                        PRE[("ffn", 0, 0)] = ffn_pieces(0, 0, 0)

                def ypart(half, W2=W2, B2=B2, gt=gt, bgt=bgt, s=s):
                    for dc in range(half * 4, half * 4 + 4):
                        _yi[0] = (_yi[0] + 1) % 4
                        Y, bY = PSB[4 + _yi[0]], BANKB[4 + _yi[0]]
                        for jj in range(2):
                            mm(Y, W2[:, jj, dc * 128:(dc + 1) * 128], gt[:, jj, :], jj == 0, jj == 1, [B2, bgt], bY)
                        xs = xT[:, dc, s * 512:(s + 1) * 512]
                        stt(xs, Y, MODS[:, idx, 32 + dc:33 + dc], xs, ALU.mult, ALU.add,
                            bY + [BMOD[idx], XB[dc][s]], [XB[dc][s]])
                pend = ypart
                it += 1
        pend(0)
        pend(1)

    def proj_tm(l, col0, func, dst, dstbufs):
        wsrc = win_d[l].rearrange("(kc p) f -> p kc f", p=128)
        pre = PRE.pop(("mixv", l), None) if col0 == 1024 else None
        if pre is not None:
            Wa, Ba, Wb, Bb = pre
        else:
            Wa, Ba = wpiece(wsrc[:, :, col0:col0 + 256], KC, 256)
            Wb, Bb = wpiece(wsrc[:, :, col0 + 256:col0 + 512], KC, 256)
        for c in range(NCH):
            ps, pb = nbank4()
            for half, (W, Bw) in enumerate(((Wa, Ba), (Wb, Bb))):
                for kc in range(KC):
                    mm(ps[:, half * 256:(half + 1) * 256], hT[:, kc, c * 128:(c + 1) * 128], W[:, kc, :],
                       kc == 0, kc == KC - 1, [Bw, HB[kc][c // 4]], [pb])
            act(dst[:, c, :, 0:128], ps.rearrange("p (h e) -> p h e", h=4), func, [pb], [dstbufs[c]])

    def head_norm_gen(src, bsrc, gate_ap, bgate, jh, c, k, rel=None):
        st, bst = ST[k], BST[k]
        P.op("dve", lambda e: e.bn_stats(st[:, 0:6], src), [bsrc], [bst])
        yield
        P.op("dve", lambda e: e.bn_aggr(st[:, 6:8], st[:, 0:6]), [bst], [bst])
        yield
        act(st[:, 8:9], st[:, 7:8], AF.Sqrt, [bst], [bst], bias=EPS, scale=1.0)
        yield
        P.op("dve", lambda e: e.reciprocal(st[:, 9:10], st[:, 8:9]), [bst], [bst])
        yield
        y1, by1 = F3[k], BF3[k]
        if gate_ap is not None:
            stt(y1[:, 0:128], src, st[:, 6:7], gate_ap, ALU.subtract, ALU.mult, [bsrc, bst, bgate], [by1])
            if rel is not None:
                pput(rel)
            yield
            act(YTM[k], y1[:, 0:128], AF.Copy, [by1, bst], [BYTM[k]], scale=st[:, 9:10])
        else:
            ts("dve", YTM[k], src, st[:, 6:7], st[:, 9:10], ALU.subtract, ALU.mult, [bsrc, bst], [BYTM[k]])
            if rel is not None:
                pput(rel)
        yield
        rg, brg, ig = pget()
        tr(rg.bitcast(BF16)[:, 0:128], YTM[k], identb, [BYTM[k], Bidb], [brg])
        yield
        cp("act", YT[:, jh, c * 128:(c + 1) * 128], rg.bitcast(BF16)[:, 0:128], [brg], [BY[jh][c]])
        pput(ig)

    BGR = [None]
    _bgc = [0]

    def run_chunks(make_gen, heads, SK):
        live = []

        def step(pred):
            nxt = []
            for cc, g in live:
                if pred(cc):
                    try:
                        next(g)
                    except StopIteration:
                        continue
                nxt.append((cc, g))
            live[:] = nxt
            _bgc[0] += 1
            if BGR[0] is not None and _bgc[0] % 6 == 0:
                next(BGR[0], None)
        for c in range(NCH):
            while any(cc < c - 1 for cc, _ in live):
                step(lambda cc: cc < c - 1)
            for hh in heads:
                live.append((c, make_gen(hh, c)))
            for _ in range(SK):
                step(lambda cc: True)
        while live:
            step(lambda cc: True)

    def retention(l):
        wsrc = win_d[l].rearrange("(kc p) f -> p kc f", p=128)
        proj_tm(l, 1024, AF.Copy, VTM, BV)
        proj_tm(l, 1536, AF.Silu, GTM, BG)
        if DBG["mix"] < 2:
            return
        for hp in range(2):
            WW = {}
            for nm, col0 in (("q", hp * 256), ("k", 512 + hp * 256)):
                sv, sbf = wload(wsrc[:, :, col0:col0 + 256], KC, 256)
                bn, bbn = wbf(KC, 256)
                bs_, bbs = wbf(KC, 256)
                wcast(bn, sv, [sbf], [bbn])
                s4 = sv.rearrange("p k (h t d) -> p k h t d", h=2, t=2)
                o4 = bs_.rearrange("p k (h t d) -> p k h t d", h=2, t=2)
                for hh in range(2):
                    cp("act", o4[:, :, hh, 0, :], s4[:, :, hh, 1, :], [sbf], [bbs])
                    cp("act", o4[:, :, hh, 1, :], s4[:, :, hh, 0, :], [sbf], [bbs])
                WW[nm] = (bn, bbn, bs_, bbs)
            for hh in range(2):
                r = hp * 2 + hh
                for nm, dstT, dbufs, tab in (("q", QT, BQ, wqtab), ("k", KT, BK, wktab)):
                    bn, bbn, bs_, bbs = WW[nm]
                    for s in range(NS):
                        pa, pba = nbank4()
                        pbk, pbb = nbank4()
                        for kc in range(KC):
                            mm(pa, bn[:, kc, hh * 128:(hh + 1) * 128], hT[:, kc, s * 512:(s + 1) * 512],
                               kc == 0, kc == KC - 1, [bbn, HB[kc][s]], [pba])
                        for kc in range(KC):
                            mm(pbk, bs_[:, kc, hh * 128:(hh + 1) * 128], hT[:, kc, s * 512:(s + 1) * 512],
                               kc == 0, kc == KC - 1, [bbs, HB[kc][s]], [pbb])
                        t1, bt1 = nsc()
                        t2, bt2 = nsc()
                        tt("dve", t1, pa, cosT[:, s * 512:(s + 1) * 512], ALU.mult, [pba, Bcos], [bt1])
                        tt("dve", t2, pbk, sinT[:, s * 512:(s + 1) * 512], ALU.mult, [pbb, Bsin], [bt2])
                        tt("pool", t1, t1, t2, ALU.add, [bt1, bt2], [bt1])
                        tb = tab[:, r * 128:(r + 1) * 128].unsqueeze(1).broadcast_to([128, 4, 128])
                        tt("pool", dstT[:, hh, s * 512:(s + 1) * 512].rearrange("p (c l) -> p c l", c=4),
                           t1.rearrange("p (c l) -> p c l", c=4), tb, ALU.mult, [bt1, Bcon], [dbufs[hh][s]])
            if DBG["mix"] < 3:
                continue
            def ret_gen(hh, c, hp=hp):
                r = hp * 2 + hh
                s = c // 4
                _ci[0] += 1
                k = _ci[0] % NSM
                gL = GAMMA[r] ** 128
                qc = QT[:, hh, c * 128:(c + 1) * 128]; kc_ = KT[:, hh, c * 128:(c + 1) * 128]
                S_ = SRET[:, l, r, :]; Sb_ = SRETB[:, l, r, :]
                rg, brg, ig = pget()
                tr(rg.bitcast(BF16)[:, 0:128], kc_, identb, [BK[hh][s], Bidb], [brg])
                yield
                act(KTM[k], rg.bitcast(BF16)[:, 0:128], AF.Copy, [brg], [BKTM[k]], scale=gL)
                pput(ig)
                rs_, brs, is_ = pget()
                mm(rs_[:, 0:128], kc_, qc, True, True, [BK[hh][s], BQ[hh][s]], [brs])
                yield
                tt("dve", SCT[k], rs_[:, 0:128], maskT, ALU.mult, [brs, Bcon], [BSCT[k]])
                pput(is_)
                yield
                ro, bro, io = pget()
                mm(ro[:, 0:128], SCT[k], VTM[:, c, r, 0:128], True, False, [BSCT[k], BV[c]], [bro])
                mm(ro[:, 0:128], qc, Sb_, False, True, [BQ[hh][s], BSb[l][r]], [bro])
                rk, brk, ik = pget()
                mm(rk[:, 0:128], KTM[k], VTM[:, c, r, 0:128], True, True, [BKTM[k], BV[c]], [brk])
                yield
                stt(S_, S_, gL, rk[:, 0:128], ALU.mult, ALU.add, [BS[l][r], brk], [BS[l][r]])
                pput(ik)
                yield
                cp("act", Sb_, S_, [BS[l][r]], [BSb[l][r]])
                yield from head_norm_gen(ro[:, 0:128], bro, GTM[:, c, r, :], BG[c], r, c, k, rel=io)
            mark(f"  ret.chunks{hp}")
            run_chunks(ret_gen, range(2), 7)
            mark(f"  ret.after{hp}")

    def mlstm(l):
        wsrc = win_d[l].rearrange("(kc p) f -> p kc f", p=128)
        if DBG["mix"] < 4:
            return
        proj_tm(l, 3072, AF.Copy, VTM, BV)
        proj_tm(l, 3584, AF.Sigmoid, GTM, BG)
        mark("  ml.gates")
        sv, sbf = wload(wsrc[:, :, 4096:4104], KC, 8)
        wg, bwg = wbf(KC, 8)
        wcast(wg, sv, [sbf], [bwg])
        gp, bgp = PSB[7], PB[7]
        for c in range(NCH):
            for t_ in range(2):
                for kc in range(KC):
                    mm(gp[:, t_ * 32 + c * 4:t_ * 32 + c * 4 + 4], hT[:, kc, c * 128:(c + 1) * 128],
                       wg[:, kc, t_ * 4:t_ * 4 + 4], kc == 0, kc == KC - 1, [bwg, HB[kc][c // 4]], [bgp])
        gtok, bgtok = nsc()
        cp("dve", gtok[:, 0:64], gp[:, 0:64], [bgp], [bgtok])
        ri, bri = nreg()
        rf, brf = nreg()
        tr(ri[0:32, 0:128], gtok[:, 0:32], identf, [bgtok, Bcon], [bri])
        tr(rf[0:32, 0:128], gtok[:, 32:64], identf, [bgtok, Bcon], [brf])
        e1, l1, nb, a_, cm = GA
        be1, bl1, bnb, ba, bcm = BGA
        mx, bmx = cm, bcm
        act(e1, rf[0:32, 0:128], AF.Exp, [brf, Bsm], [be1], bias=negbf[l], scale=-1.0)
        act(l1, e1, AF.Ln, [be1], [bl1], bias=1.0, scale=1.0)
        scan(nb, onesf[0:32, :], l1, 0.0, ALU.mult, ALU.add, [Bcon, bl1], [bnb])
        stt(a_, ri[0:32, 0:128], bi32[l], nb, ALU.add, ALU.add, [bri, Bvec, bnb], [ba])
        scan(cm, zeros32, a_, -1e30, ALU.add, ALU.max, [Bz32, ba], [bcm])
        tt("dve", GS[:, 0:1], cm[:, 127:128], nb[:, 127:128], ALU.subtract, [bcm, bnb], [BGS])
        ts("dve", GS[:, 1:2], nb[:, 127:128], -1.0, None, ALU.mult, None, [bnb], [BGS])
        rr, brr = nreg()
        tr(rr[0:1, 0:32], GS[:, 0:1], identf[0:32, 0:32], [BGS, Bcon], [brr])
        tr(rr[0:1, 32:64], GS[:, 1:2], identf[0:32, 0:32], [BGS, Bcon], [brr])
        cp("dve", ROW[:, 0:2, :], rr[0:1, 0:64].rearrange("p (a b) -> p a b", a=2), [brr], [BROW])
        rowv = lambda i: ROW[:, i, :].rearrange("p (c h) -> p c h", h=4)
        for h in range(4):
            scan(rowv(3)[:, :, h], rowv(1)[:, :, h], rowv(0)[:, :, h], MCAR[:, l, h:h + 1], ALU.add, ALU.max,
                 [BROW, BMC[l]], [BROW])
        cp("dve", ROW[:, 2, 4:32], ROW[:, 3, 0:28], [BROW], [BROW])
        cp("dve", ROW[:, 2, 0:4], MCAR[:, l, :], [BROW, BMC[l]], [BROW])
        cp("dve", MCAR[:, l, :], ROW[:, 3, 28:32], [BROW], [BMC[l]])
        rc, brc = nreg()
        mm(rc[0:32, 0:1], ROW[:, 2, :], onesf[0:1, 0:1], True, True, [BROW, Bcon], [brc])
        mm(rc[0:32, 1:2], ROW[:, 3, :], onesf[0:1, 0:1], True, True, [BROW, Bcon], [brc])
        cp("dve", GS[:, 2:4], rc[0:32, 0:2], [brc], [BGS])
        ts("dve", GS[:, 4:5], GS[:, 2:3], LNS, None, ALU.add, None, [BGS], [BGS])
        tt("dve", GS[:, 5:6], GS[:, 1:2], GS[:, 3:4], ALU.subtract, [BGS], [BGS])
        tt("dve", GS[:, 6:7], GS[:, 5:6], GS[:, 2:3], ALU.add, [BGS], [BGS])
        act(GS[:, 7:8], GS[:, 6:7], AF.Exp, [BGS], [BGS])
        ts("dve", mx, cm, GS[:, 2:3], None, ALU.max, None, [bcm, BGS], [bmx])
        rq, brq = nreg()

        def qexp(q, src, rbufs, **kw):
            act(QTT[:, q % 2, :], src, AF.Exp, rbufs, [BQTT[q % 2]], **kw)
            tr(rq[:, q * 32:(q + 1) * 32], QTT[:, q % 2, :], identf[0:32, 0:32], [BQTT[q % 2], Bcon], [brq])
        qexp(0, a_, [ba])
        qexp(1, mx, [bmx], bias=LNS, scale=-1.0)
        qexp(2, mx, [bmx, BGS], bias=GS[:, 4:5], scale=-1.0)
        tt("dve", e1, nb, mx, ALU.subtract, [bnb, bmx], [be1])
        qexp(3, e1, [be1])
        qexp(4, a_, [ba, BGS], bias=GS[:, 5:6], scale=1.0)
        cp("dve", GSC, rq[:, 0:160].rearrange("p (q j) -> p q j", q=5), [brq], [BGSC])
        ts("dve", WCS, onesf[0:32, :], GS[:, 7:8], None, ALU.mult, None, [Bcon, BGS], [BWCS])
        rw, brw = nreg()
        mm(rw[:, 0:32], WCS, identf[0:32, 0:32], True, True, [BWCS, Bcon], [brw])
        cp("dve", WCB, rw[:, 0:32], [brw], [BWCB])
        mark("  ml.pairs")
        if DBG["mix"] < 5:
            return
        for hp in range(2):
            WQ, BWQ = wpiece(wsrc[:, :, 2048 + hp * 256:2048 + (hp + 1) * 256], KC, 256)
            WK, BWK = wpiece(wsrc[:, :, 2560 + hp * 256:2560 + (hp + 1) * 256], KC, 256)
            for hh in range(2):
                h = hp * 2 + hh
                for j, Wp, Bp, dstT, dbufs in ((h, WQ, BWQ, QT, BQ), (4 + h, WK, BWK, KT, BK)):
                    ub, bub = BIG[j // 4], BBIG[j // 4]
                    cp("pool", ub[:, 0:3], CCAR[:, l, j, 0:3], [BCC[l][j]], [bub])
                    for s in range(NS):
                        ps, pb = nbank4()
                        for kc in range(KC):
                            mm(ps, Wp[:, kc, hh * 128:(hh + 1) * 128], hT[:, kc, s * 512:(s + 1) * 512],
                               kc == 0, kc == KC - 1, [Bp, HB[kc][s]], [pb])
                        cp("act", ub[:, 3 + s * 512:3 + (s + 1) * 512], ps, [pb], [bub])
                    cp("pool", CCAR[:, l, j, 0:3], ub[:, T:T + 3], [bub], [BCC[l][j]])
                    for s in range(NS):
                        ac, bac = nsc()
                        lo = s * 512
                        cw = lambda t, j=j: convw[:, (l * 4 + t) * 8 + j:(l * 4 + t) * 8 + j + 1]
                        ts("dve", ac, ub[:, lo:lo + 512], cw(0), None, ALU.mult, None, [bub, Bvec], [bac])
                        for t in range(1, 4):
                            stt(ac, ub[:, lo + t:lo + t + 512], cw(t), ac, ALU.mult, ALU.add, [bub, Bvec, bac], [bac])
                        act(dstT[:, hh, lo:lo + 512], ac, AF.Silu, [bac, Bvec], [dbufs[hh][s]],
                            bias=convb[:, l * 8 + j:l * 8 + j + 1], scale=1.0)
            if DBG["mix"] < 6:
                continue
            def ml_gen(hh, c, hp=hp):
                h = hp * 2 + hh
                s = c // 4
                _ci[0] += 1
                k = _ci[0] % NSM
                jc = c * 4 + h
                qc = QT[:, hh, c * 128:(c + 1) * 128]; kc_ = KT[:, hh, c * 128:(c + 1) * 128]
                C_ = CN[:, l, h, :]; Cb_ = CNB[:, l, h, :]
                f1, bf1 = F1[k], BF1[k]
                f2, bf2 = F2[k], BF2[k]
                st, bst = ST[k], BST[k]
                rg, brg, ig = pget()
                tr(rg.bitcast(BF16)[:, 0:128], kc_, identb, [BK[hh][s], Bidb], [brg])
                yield
                act(KTM[k], rg.bitcast(BF16)[:, 0:128], AF.Copy, [brg, BGSC], [BKTM[k]], scale=GSC[:, 4, jc:jc + 1])
                pput(ig)
                rs_, brs, is_ = pget()
                mm(rs_[:, 0:128], kc_, qc, True, True, [BK[hh][s], BQ[hh][s]], [brs])
                yield
                stt(SCT[k], rs_[:, 0:128], GSC[:, 0, jc:jc + 1], maskT, ALU.mult, ALU.mult, [brs, BGSC, Bcon], [BSCT[k]])
                pput(is_)
                yield
                r2, br2, i2 = pget()
                mm(r2[:, 0:129], qc, Cb_, True, True, [BQ[hh][s], BCb[l][h]], [br2])
                rk, brk, ik = pget()
                mm(rk[:, 0:129], KTM[k], VTM[:, c, h, :], True, True, [BKTM[k], BV[c]], [brk])
                yield
                act(f1[:, 0:129], r2[:, 0:129], AF.Copy, [br2, BGSC], [bf1], scale=GSC[:, 2, jc:jc + 1])
                pput(i2)
                stt(C_, C_, WCB[:, jc:jc + 1], rk[:, 0:129], ALU.mult, ALU.add, [BC[l][h], BWCB, brk], [BC[l][h]])
                pput(ik)
                r1, br1, i1 = pget()
                mm(r1[:, 0:129], SCT[k], VTM[:, c, h, :], True, True, [BSCT[k], BV[c]], [br1])
                yield
                cp("act", Cb_, C_, [BC[l][h]], [BCb[l][h]])
                stt(f2[:, 0:129], r1[:, 0:129], GSC[:, 1, jc:jc + 1], f1[:, 0:129], ALU.mult, ALU.add,
                    [br1, BGSC, bf1], [bf2])
                pput(i1)
                yield
                act(st[:, 10:11], f2[:, 128:129], AF.Abs, [bf2], [bst])
                yield
                tt("dve", st[:, 11:12], st[:, 10:11], GSC[:, 3, jc:jc + 1], ALU.max, [bst, BGSC], [bst])
                yield
                P.op("dve", lambda e, o_=st[:, 12:13], i_=st[:, 11:12]: e.reciprocal(o_, i_), [bst], [bst])
                yield
                stt(f1[:, 0:128], f2[:, 0:128], st[:, 12:13], GTM[:, c, h, :], ALU.mult, ALU.mult,
                    [bf2, bst, BG[c]], [bf1])
                yield
                yield from head_norm_gen(f1[:, 0:128], bf1, None, None, 4 + h, c, k)
            mark(f"  ml.chunks{hp}")
            run_chunks(ml_gen, range(2), 9)
            mark(f"  ml.after{hp}")

    def mixer(l, ti):
        idx = l * 3 + 1
        mark(f"t{ti}.mix{l}.norm")
        norm_mod(idx)
        mark(f"t{ti}.mix{l}.ret")
        if ti == 0:
            BGR[0] = mods_gen([2, 3, 4] if l == 0 else [5])
        retention(l)
        mark(f"t{ti}.mix{l}.mlstm")
        mlstm(l)
        if BGR[0] is not None:
            for _ in BGR[0]:
                pass
            BGR[0] = None
        mark(f"t{ti}.mix{l}.wout")
        if DBG["mix"] < 7:
            return
        WO = []
        for pj in range(4):
            sv, sbf = wload(wout_d[l, pj * 256:(pj + 1) * 256, :].rearrange("(jj p) d -> p jj d", p=128), 2, D)
            bv, bbv = wbf(2, D)
            for jj in range(2):
                jidx = l * 8 + pj * 2 + jj
                act(bv[:, jj, :], sv[:, jj, :], AF.Copy, [sbf, Bvec], [bbv], scale=gmix[:, jidx:jidx + 1])
            WO.append((bv, bbv))
        for s in range(NS):
            for dc in range(KC):
                Y, bY = nbank4()
                for j in range(8):
                    bv, bbv = WO[j // 2]
                    mm(Y, bv[:, j % 2, dc * 128:(dc + 1) * 128], YT[:, j, s * 512:(s + 1) * 512], j == 0, j == 7,
                       [bbv] + [BY[j][c] for c in range(s * 4, s * 4 + 4)], [bY])
                xs = xT[:, dc, s * 512:(s + 1) * 512]
                stt(xs, Y, MODS[:, idx, 32 + dc:33 + dc], xs, ALU.mult, ALU.add, [bY, BMOD[idx], XB[dc][s]], [XB[dc][s]])
        PRE[("ffn", l, 1)] = ffn_pieces(l, 1, 0)

    def store_out(ti, final):
        rsl = []
        for s in range(NS):
            if final:
                rs, brs = rms_stats(s)
            for kc in range(KC):
                xs = xT[:, kc, s * 512:(s + 1) * 512]
                if final:
                    stt(xs, xs, gfin[:, kc:kc + 1], rs, ALU.mult, ALU.mult, [XB[kc][s], Bvec, brs], [XB[kc][s]])
        for blk in range(NCH):
            s = blk // 4
            si, xo, bxo = stg_slot()
            for k4 in range(2):
                ps, pb = nbank4()
                for q in range(4):
                    kc = k4 * 4 + q
                    tr(ps[:, q * 128:(q + 1) * 128], xT[:, kc, blk * 128:(blk + 1) * 128], identf,
                       [XB[kc][s], Bcon], [pb])
                cp("act" if k4 else "dve", xo[:, k4 * 512:(k4 + 1) * 512], ps, [pb], [bxo])
            dma(f"stg{si}", out_d[ti * T + blk * 128: ti * T + (blk + 1) * 128, :], xo, [bxo], [])

    for ti in range(nt_run):
        mark(f"t{ti}.load")
        load_x(ti)
        rotary_tables(ti)
        st = 0
        for l in range(2):
            for sub in range(3):
                st += 1
                if st > stage:
                    break
                if sub == 0:
                    ffn(l, 0, ti)
                elif sub == 1:
                    mixer(l, ti)
                else:
                    ffn(l, 1, ti)
        mark(f"t{ti}.store")
        store_out(ti, final=(stage >= 7))
    mark("end")
    P.emit()
    return nc, P


_CACHE = {}


def kernel(**inputs):
    inp = {k: np.asarray(v) for k, v in inputs.items()}
    if "nc" not in _CACHE:
        _CACHE["nc"] = build()[0]
    nc = _CACHE["nc"]
    con = make_consts()
    shared = {k: np.ascontiguousarray(inp[k], dtype=np.float32)
              for k in ("w_ada", "w_ff1", "w_ff3", "w_ff2", "w_in", "w_out")}
    in_maps = []
    for b in range(NB):
        m = dict(shared)
        m["x"] = np.ascontiguousarray(inp["x"][b], dtype=np.float32)
        m["pos"] = np.ascontiguousarray(inp["positions"][b].reshape(1, SEQ), dtype=np.int32)
        m["vecs"] = make_vecs(inp["c"][b], inp)
        m["consts"] = con
        in_maps.append(m)
    res = run_bass_kernel_spmd(nc, in_maps, core_ids=list(range(NB)))
    return np.stack([np.asarray(res.results[b]["out"], dtype=np.float32) for b in range(NB)], axis=0)
```
